# Optimizing a Trainium2 kernel written in Bass

```python
import jax, jax.numpy as jnp
from jax import lax
import numpy as np

D_MODEL = 1024
BATCH = 2
SEQ = 8192
DEPTH = 1

SWA_HEADS = 8
SWA_KV_HEADS = 2
SWA_HEAD_DIM = 64
WINDOW = 128
ATTN_BLOCK = 128
MLA_HEADS = 8
MLA_Q_RANK = 384
MLA_KV_RANK = 256
MLA_NOPE_DIM = 64
MLA_ROPE_DIM = 32
MLA_V_DIM = 64
ROPE_THETA = 10000.0
N_EXPERTS = 32
TOP_K = 4
D_EXPERT = 1024
SWIGLU_LIMIT = 7.0
SWIGLU_ALPHA = 1.702
MOE_BLOCK = 128
NORM_EPS = 1e-6

SWA_Q_DIM = SWA_HEADS * SWA_HEAD_DIM
SWA_KV_DIM = SWA_KV_HEADS * SWA_HEAD_DIM
MLA_QK_DIM = MLA_NOPE_DIM + MLA_ROPE_DIM
MLA_OUT_DIM = MLA_HEADS * MLA_V_DIM
IN_WIDTHS = (SWA_Q_DIM, SWA_KV_DIM, SWA_KV_DIM, MLA_Q_RANK, MLA_KV_RANK, MLA_ROPE_DIM, D_MODEL, D_MODEL)
IN_DIM = 3488

kernel_name = 'hybrid_swa_mla_moe_adaln_block'


def rmsnorm(x, g):
    x32 = x.astype(jnp.float32)
    r = x32 * lax.rsqrt(jnp.mean(x32 * x32, axis=-1, keepdims=True) + NORM_EPS)
    return (r * g.astype(jnp.float32)).astype(x.dtype)


def alibi_slopes(n_heads):
    return jnp.asarray(np.exp2(-8.0 * np.arange(1, n_heads + 1) / n_heads), dtype=jnp.float32)


def rope(t, cos, sin):
    half = t.shape[-1] // 2
    t1, t2 = t[..., :half], t[..., half:]
    return jnp.concatenate([t1 * cos - t2 * sin, t2 * cos + t1 * sin], axis=-1)


def sliding_window_attention(q, k, v, sinks):
    B, S = q.shape[:2]
    nb = S // ATTN_BLOCK
    G = SWA_HEADS // SWA_KV_HEADS
    qb = q.reshape(B, nb, ATTN_BLOCK, SWA_KV_HEADS, G, SWA_HEAD_DIM)

    def band(t):
        tp = jnp.pad(t, ((0, 0), (ATTN_BLOCK, 0), (0, 0), (0, 0)))
        tp = tp.reshape(B, nb + 1, ATTN_BLOCK, SWA_KV_HEADS, SWA_HEAD_DIM)
        return jnp.concatenate([tp[:, :-1], tp[:, 1:]], axis=2)

    kw, vw = band(k), band(v)
    scores = jnp.einsum('bnqhgd,bnkhd->bnhgqk', qb, kw).astype(jnp.float32) * (SWA_HEAD_DIM ** -0.5)
    qi = jnp.arange(ATTN_BLOCK)[:, None]
    kj = jnp.arange(2 * ATTN_BLOCK)[None, :]
    dist = qi - kj + ATTN_BLOCK
    key_abs = jnp.arange(nb)[:, None, None] * ATTN_BLOCK - ATTN_BLOCK + kj[None]
    valid = (dist >= 0) & (dist < WINDOW) & (key_abs >= 0)
    slopes = alibi_slopes(SWA_HEADS).reshape(SWA_KV_HEADS, G)
    scores = scores - slopes[:, :, None, None] * dist.astype(jnp.float32)
    scores = jnp.where(valid[None, :, None, None], scores, -jnp.inf)
    sink = jnp.broadcast_to(sinks.astype(jnp.float32).reshape(SWA_KV_HEADS, G)[:, :, None, None],
                            scores.shape[:-1] + (1,))
    probs = jax.nn.softmax(jnp.concatenate([scores, sink], axis=-1), axis=-1)[..., :-1]
    out = jnp.einsum('bnhgqk,bnkhd->bnqhgd', probs.astype(v.dtype), vw)
    return out.reshape(B, S, SWA_Q_DIM)


def mla_attention(q_nope, q_rope, k_nope, k_rope, v):
    B, S = q_nope.shape[:2]
    nb = S // ATTN_BLOCK
    scale = MLA_QK_DIM ** -0.5
    key_pos = jnp.arange(S)

    def blocks(t):
        return jnp.moveaxis(t.reshape((B, nb, ATTN_BLOCK) + t.shape[2:]), 1, 0)

    def attend(args):
        qn, qr, n = args
        s = (jnp.einsum('bqhd,bkhd->bhqk', qn, k_nope).astype(jnp.float32)
             + jnp.einsum('bqhd,bkd->bhqk', qr, k_rope).astype(jnp.float32)) * scale
        q_pos = n * ATTN_BLOCK + jnp.arange(ATTN_BLOCK)
        s = jnp.where(key_pos[None, :] <= q_pos[:, None], s, -jnp.inf)
        p = jax.nn.softmax(s, axis=-1)
        return jnp.einsum('bhqk,bkhd->bqhd', p.astype(v.dtype), v)

    out = lax.map(attend, (blocks(q_nope), blocks(q_rope), jnp.arange(nb)))
    return jnp.moveaxis(out, 0, 1).reshape(B, S, MLA_OUT_DIM)


def clamped_swiglu(hb):
    x_glu = jnp.minimum(hb[..., :D_EXPERT], SWIGLU_LIMIT)
    x_lin = jnp.clip(hb[..., D_EXPERT:], -SWIGLU_LIMIT, SWIGLU_LIMIT)
    return x_glu * jax.nn.sigmoid(SWIGLU_ALPHA * x_glu) * (x_lin + 1.0)


def moe(h, w_router, b_router, w1, b1, w2, b2):
    B, S, D = h.shape
    xt = h.reshape(-1, D)
    N = xt.shape[0]
    logits = (xt @ w_router + b_router).astype(jnp.float32)
    top_vals, top_idx = lax.top_k(logits, TOP_K)
    top_w = jax.nn.softmax(top_vals, axis=-1)
    NK = N * TOP_K
    flat_e = top_idx.reshape(-1)
    flat_tok = jnp.repeat(jnp.arange(N, dtype=jnp.int32), TOP_K)
    flat_w = top_w.reshape(-1)
    order = jnp.argsort(flat_e)
    sorted_e = flat_e[order]
    counts = jnp.bincount(flat_e, length=N_EXPERTS)
    padded = ((counts + MOE_BLOCK - 1) // MOE_BLOCK) * MOE_BLOCK
    starts = jnp.cumsum(counts) - counts
    pends = jnp.cumsum(padded)
    pstarts = pends - padded
    dest = pstarts[sorted_e] + (jnp.arange(NK) - starts[sorted_e])
    P = NK + N_EXPERTS * MOE_BLOCK
    n_blocks = P // MOE_BLOCK
    tok_buf = jnp.zeros((P,), jnp.int32).at[dest].set(flat_tok[order])
    w_buf = jnp.zeros((P,), h.dtype).at[dest].set(flat_w[order].astype(h.dtype))
    block_e = jnp.minimum(jnp.searchsorted(pends, jnp.arange(n_blocks) * MOE_BLOCK, side='right'),
                          N_EXPERTS - 1)

    def expert_block(args):
        e, toks = args
        xb = xt[toks]
        hb = clamped_swiglu(xb @ w1[e] + b1[e])
        return hb @ w2[e] + b2[e]

    ys = lax.map(expert_block, (block_e, tok_buf.reshape(n_blocks, MOE_BLOCK)))
    y = jnp.zeros_like(xt).at[tok_buf].add(ys.reshape(P, D) * w_buf[:, None])
    return y.reshape(B, S, D)


def setup_inputs(seed: int = 0) -> dict:
    key = jax.random.key(seed)
    ks = jax.random.split(key, 26)
    f32 = jnp.float32

    def nrm(k, shape, fan_in, mult=1.0):
        return jax.random.normal(k, shape, f32) * (mult * fan_in ** -0.5)

    L, D, E, F = DEPTH, D_MODEL, N_EXPERTS, D_EXPERT
    return {
        'x': jax.random.normal(ks[0], (BATCH, SEQ, D), f32),
        'c': jax.random.normal(ks[1], (BATCH, D), f32),
        'positions': jax.random.randint(ks[2], (BATCH, 1), 0, 1024, jnp.int32)
                     + jnp.arange(SEQ, dtype=jnp.int32)[None, :],
        'w_ada': nrm(ks[3], (L, D, 6 * D), D, 0.5),
        'b_ada': 0.01 * jax.random.normal(ks[4], (L, 6 * D), f32),
        'norm_mix': 1.0 + 0.05 * jax.random.normal(ks[5], (L, D), f32),
        'norm_ffn': 1.0 + 0.05 * jax.random.normal(ks[6], (L, D), f32),
        'w_in': nrm(ks[7], (L, D, IN_DIM), D),
        'sinks': 0.5 * jax.random.normal(ks[8], (L, SWA_HEADS), f32),
        'q_norm': 1.0 + 0.05 * jax.random.normal(ks[9], (L, MLA_Q_RANK), f32),
        'kv_norm': 1.0 + 0.05 * jax.random.normal(ks[10], (L, MLA_KV_RANK), f32),
        'w_uq': nrm(ks[11], (L, MLA_Q_RANK, MLA_HEADS * MLA_QK_DIM), MLA_Q_RANK),
        'w_uk': nrm(ks[12], (L, MLA_KV_RANK, MLA_HEADS * MLA_NOPE_DIM), MLA_KV_RANK),
        'w_uv': nrm(ks[13], (L, MLA_KV_RANK, MLA_OUT_DIM), MLA_KV_RANK),
        'w_branch_a': nrm(ks[14], (L, SWA_Q_DIM, D), SWA_Q_DIM),
        'w_branch_b': nrm(ks[15], (L, MLA_OUT_DIM, D), MLA_OUT_DIM),
        'w_out': nrm(ks[16], (L, D, D), D),
        'w_router': nrm(ks[17], (L, D, E), D),
        'b_router': 0.01 * jax.random.normal(ks[18], (L, E), f32),
        'w_moe1': nrm(ks[19], (L, E, D, 2 * F), D),
        'b_moe1': 0.01 * jax.random.normal(ks[20], (L, E, 2 * F), f32),
        'w_moe2': nrm(ks[21], (L, E, F, D), F),
        'b_moe2': 0.01 * jax.random.normal(ks[22], (L, E, D), f32),
        'final_norm': 1.0 + 0.05 * jax.random.normal(ks[23], (D,), f32),
    }


def reference(x, c, positions, w_ada, b_ada, norm_mix, norm_ffn, w_in, sinks, q_norm, kv_norm,
              w_uq, w_uk, w_uv, w_branch_a, w_branch_b, w_out, w_router, b_router,
              w_moe1, b_moe1, w_moe2, b_moe2, final_norm):
    B, S, D = x.shape
    freqs = ROPE_THETA ** (-jnp.arange(0, MLA_ROPE_DIM, 2, dtype=jnp.float32) / MLA_ROPE_DIM)
    ang = positions.astype(jnp.float32)[..., None] * freqs
    cos, sin = jnp.cos(ang).astype(x.dtype), jnp.sin(ang).astype(x.dtype)
    split_at = list(np.cumsum(IN_WIDTHS)[:-1])
    c_act = jax.nn.silu(c)

    for l in range(DEPTH):
        mod = c_act @ w_ada[l] + b_ada[l]
        sh1, sc1, g1, sh2, sc2, g2 = [m[:, None, :] for m in jnp.split(mod, 6, axis=-1)]

        h = rmsnorm(x, norm_mix[l]) * (1.0 + sc1) + sh1
        proj = h @ w_in[l]
        qa, ka, va, cq, ckv, kr, gate_a, gate_b = jnp.split(proj, split_at, axis=-1)

        ya = sliding_window_attention(qa.reshape(B, S, SWA_HEADS, SWA_HEAD_DIM),
                                      ka.reshape(B, S, SWA_KV_HEADS, SWA_HEAD_DIM),
                                      va.reshape(B, S, SWA_KV_HEADS, SWA_HEAD_DIM),
                                      sinks[l])
        ya = ya @ w_branch_a[l]

        q = (rmsnorm(cq, q_norm[l]) @ w_uq[l]).reshape(B, S, MLA_HEADS, MLA_QK_DIM)
        q_nope = q[..., :MLA_NOPE_DIM]
        q_rope = rope(q[..., MLA_NOPE_DIM:], cos[:, :, None, :], sin[:, :, None, :])
        ckv_n = rmsnorm(ckv, kv_norm[l])
        k_nope = (ckv_n @ w_uk[l]).reshape(B, S, MLA_HEADS, MLA_NOPE_DIM)
        v_b = (ckv_n @ w_uv[l]).reshape(B, S, MLA_HEADS, MLA_V_DIM)
        k_rope = rope(kr, cos, sin)
        yb = mla_attention(q_nope, q_rope, k_nope, k_rope, v_b) @ w_branch_b[l]

        mixed = jax.nn.sigmoid(gate_a) * ya + jax.nn.sigmoid(gate_b) * yb
        x = x + g1 * (mixed @ w_out[l])

        h2 = rmsnorm(x, norm_ffn[l]) * (1.0 + sc2) + sh2
        x = x + g2 * moe(h2, w_router[l], b_router[l], w_moe1[l], b_moe1[l], w_moe2[l], b_moe2[l])

    return rmsnorm(x, final_norm)
```

```python
import numpy as np
import concourse.bass as bass
import concourse.mybir as mybir
from concourse.bass_utils import run_bass_kernel_spmd
from contextlib import ExitStack

F32 = mybir.dt.float32
BF16 = mybir.dt.bfloat16
I32 = mybir.dt.int32
AF = mybir.ActivationFunctionType
ALU = mybir.AluOpType

ENGS = ('pe', 'act', 'dve', 'pool', 'sp')
SEM_LIMIT = 12000
NEG = -30000.0
D = 1024
S = 8192
NCORES = 8
EPS = 1e-6
TWO_PI = 6.283185307179586
PI = 3.141592653589793


class Buf:
    __slots__ = ('w', 'r')

    def __init__(self):
        self.w = None
        self.r = []


def bufs(n):
    return [Buf() for _ in range(n)]


class Prog:
    def __init__(self, nc, stack, n_dma_sems=48):
        self.nc = nc
        self.stack = stack
        self.streams = {e: [] for e in ENGS}
        self.cur = {}
        self.cnt = {}
        self.last = {e: None for e in ENGS}
        self.known = {e: {} for e in ENGS}
        self.nsem = 0
        for e in ENGS:
            self._new_eng_sem(e)
        self.dma_sems = []
        for i in range(n_dma_sems):
            s = stack.enter_context(nc.semaphore(f"dq{i}"))
            self.dma_sems.append([s, 0, None])
        self.dma_rr = 0

    def _new_eng_sem(self, e):
        s = self.stack.enter_context(self.nc.semaphore(f"s_{e}_{self.nsem}"))
        self.nsem += 1
        self.cur[e] = s
        self.cnt[e] = 0

    def _waits_for(self, e, deps):
        need = {}
        for tok in deps:
            if tok is None:
                continue
            sem, val = tok
            k = id(sem)
            if k not in need or need[k][1] < val:
                need[k] = (sem, val)
        waits = []
        kn = self.known[e]
        for k, (sem, val) in need.items():
            if kn.get(k, 0) < val:
                waits.append((sem, val))
                kn[k] = val
        return waits

    @staticmethod
    def _deps(reads, writes):
        deps = []
        for b in reads:
            deps.append(b.w)
        for b in writes:
            deps.append(b.w)
            deps.extend(b.r)
        return deps

    @staticmethod
    def _mark(tok, reads, writes):
        for b in reads:
            b.r = [t for t in b.r if t[0] is not tok[0]] + [tok]
        for b in writes:
            b.w = tok
            b.r = []

    def op(self, e, fn, reads=(), writes=()):
        deps = self._deps(reads, writes)
        if e == 'pe':
            deps = [t for t in deps if t is not None and t[0] is not self.cur['pe']]
        waits = self._waits_for(e, deps)
        if self.cnt[e] >= SEM_LIMIT:
            self._new_eng_sem(e)
        self.cnt[e] += 1
        tok = (self.cur[e], self.cnt[e])
        self.last[e] = tok
        self.streams[e].append((waits, fn, tok, 1))
        self._mark(tok, reads, writes)
        return tok

    def dma(self, q, out, in_, reads=(), writes=()):
        deps = self._deps(reads, writes)
        ent = self.dma_sems[self.dma_rr]
        self.dma_rr = (self.dma_rr + 1) % len(self.dma_sems)
        if ent[2] is not None:
            deps.append(ent[2])
        waits = self._waits_for(q, deps)
        ent[1] += 16
        tok = (ent[0], ent[1])
        ent[2] = tok
        fn = lambda eng, o=out, i=in_: eng.dma_start(out=o, in_=i)
        self.streams[q].append((waits, fn, tok, 16))
        self._mark(tok, reads, writes)
        return tok

    def wait_all(self, e, toks):
        waits = self._waits_for(e, toks)
        if waits:
            self.streams[e].append((waits, None, None, 0))

    def barrier(self):
        toks = [self.last[e] for e in ENGS] + [ent[2] for ent in self.dma_sems]
        for e in ENGS:
            self.wait_all(e, toks)

    def finish(self):
        nc = self.nc
        with nc.Block() as block:
            def run(eng, items):
                for waits, fn, tok, inc in items:
                    for sem, val in waits:
                        eng.wait_ge(sem, val)
                    if fn is not None:
                        fn(eng).then_inc(tok[0], inc)

            @block.tensor
            def _(eng):
                run(eng, self.streams['pe'])

            @block.scalar
            def _(eng):
                run(eng, self.streams['act'])

            @block.vector
            def _(eng):
                run(eng, self.streams['dve'])

            @block.gpsimd
            def _(eng):
                run(eng, self.streams['pool'])

            @block.sync
            def _(eng):
                run(eng, self.streams['sp'])


class Ops:
    def __init__(self, P):
        self.P = P

    def mm(self, out, lhsT, rhs, start, stop, R, W):
        return self.P.op('pe', lambda e: e.matmul(out, lhsT, rhs, start=start, stop=stop), R, W)

    def tr(self, out, in_, ident, R, W):
        return self.P.op('pe', lambda e: e.transpose(out, in_, ident), R, W)

    def act(self, out, in_, func, R, W, **kw):
        return self.P.op('act', lambda e: e.activation(out=out, in_=in_, func=func, **kw), R, W)

    def ts(self, eng, out, in0, s1, s2, op0, op1, R, W, **kw):
        if op1 is None:
            return self.P.op(eng, lambda e: e.tensor_scalar(out, in0, s1, None, op0, **kw), R, W)
        return self.P.op(eng, lambda e: e.tensor_scalar(out, in0, s1, s2, op0, op1, **kw), R, W)

    def tt(self, eng, out, in0, in1, op, R, W):
        return self.P.op(eng, lambda e: e.tensor_tensor(out, in0, in1, op), R, W)

    def stt(self, out, in0, scalar, in1, op0, op1, R, W, **kw):
        return self.P.op('dve', lambda e: e.scalar_tensor_tensor(out, in0, scalar, in1, op0, op1, **kw), R, W)

    def cp(self, eng, out, in_, R, W):
        if eng == 'act':
            return self.P.op('act', lambda e: e.copy(out, in_), R, W)
        return self.P.op(eng, lambda e: e.tensor_copy(out, in_), R, W)

    def memset(self, eng, ap, val, W):
        return self.P.op(eng, lambda e: e.memset(ap, val), (), W)

    def recip(self, out, in_, R, W):
        return self.P.op('dve', lambda e: e.reciprocal(out, in_), R, W)

    def max8(self, out, in_, R, W):
        return self.P.op('dve', lambda e: e.max(out, in_), R, W)

    def dma(self, q, out, in_, R, W):
        return self.P.dma(q, out, in_, R, W)


IN_SPECS = [
    ("xq", [4, 640, D], F32), ("xall", [S, D], F32),
    ("posq", [128, 16], I32), ("posall", [128, 64], I32),
    ("qidx", [128, 2048], F32), ("kidx", [128, 64], F32), ("halo_bias", [128, 16], F32),
    ("c_col", [128, 8], F32), ("w_ada", [D, 6 * D], F32), ("b_ada_b", [128, 6 * D], F32),
    ("norm_mix_col", [128, 8], F32), ("norm_ffn_b", [128, D], F32), ("final_norm_b", [128, D], F32),
    ("w_in", [D, 3488], F32), ("sinks_b", [128, 8], F32),
    ("q_norm_col", [128, 3], F32), ("kv_norm_col", [128, 2], F32),
    ("w_uq", [384, 768], F32), ("w_uk", [256, 512], F32), ("w_uv", [256, 512], F32),
    ("w_ba", [512, D], F32), ("w_bb", [512, D], F32), ("w_out", [D, D], F32),
    ("w_router", [D, 32], F32), ("b_router_b", [128, 32], F32),
    ("w_moe1", [32, D, 2048], F32), ("b1c", [32, 128, 16], F32),
    ("w_moe2", [32, D, D], F32), ("b_moe2", [32, D], F32),
    ("ident", [128, 128], F32), ("ut", [128, 128], F32), ("iota_j", [128, 256], F32),
    ("swa_tab", [128, 2 * 8 * 128], F32), ("freq_b", [128, 64], F32),
]


def build_program(n_experts=32, phases=6):
    nc = bass.Bass("TRN2", target_bir_lowering=False)
    A = {}
    for name, shape, dt in IN_SPECS:
        A[name] = nc.dram_tensor(name, shape, dt, kind="ExternalInput").ap()
    out_d = nc.dram_tensor("out", [2048, D], F32, kind="ExternalOutput").ap()
    za_d = nc.dram_tensor("za_d", [16, 128, D], BF16, kind="Internal").ap()
    qT_d = nc.dram_tensor("qT_d", [8, 96, 2048], BF16, kind="Internal").ap()
    ckvT_d = nc.dram_tensor("ckvT_d", [2, 128, S], BF16, kind="Internal").ap()
    krT_d = nc.dram_tensor("krT_d", [32, S], BF16, kind="Internal").ap()
    obT_d = nc.dram_tensor("obT_d", [8, 64, 2048], BF16, kind="Internal").ap()
    Bza_d, BqT_d, Bckv_d, Bkr_d, Bob_d = bufs(5)
    SCALE_MLA = 96.0 ** -0.5

    with ExitStack() as gs:
        P = Prog(nc, gs)
        O = Ops(P)

        _cnt = [0]

        def sb(st, name, shape, dt):
            _cnt[0] += 1
            return st.enter_context(nc.sbuf_tensor(f"sb{_cnt[0]}_{name}", shape, dt))

        ps = [gs.enter_context(nc.psum_tensor(f"ps{i}", [128, 512], F32)) for i in range(8)]
        psb = bufs(8)

        ident_f = sb(gs, "ident_f", [128, 128], F32)
        ident_b = sb(gs, "ident_b", [128, 128], BF16)
        ones_b = sb(gs, "ones_b", [128, 128], BF16)
        Bc = Buf()
        O.dma('sp', ident_f[:], A["ident"], (), [Bc])
        O.cp('dve', ident_b[:], ident_f[:], [Bc], [Bc])
        O.memset('dve', ones_b[:], 1.0, [Bc])

        g1B = sb(gs, "g1B", [128, D], F32)
        G2B = sb(gs, "G2B", [128, D], F32)
        sh2B = sb(gs, "sh2B", [128, D], F32)
        g2B = sb(gs, "g2B", [128, D], F32)
        G1col = sb(gs, "G1col", [128, 8], F32)
        sh1col = sb(gs, "sh1col", [128, 8], BF16)
        Bmod = Buf()
        xt = [sb(gs, f"xt{i}", [128, D], F32) for i in range(2)]
        xtb = bufs(2)
        xh = sb(gs, "xh", [128, D], BF16)
        xhT = sb(gs, "xhT", [128, 8, 128], BF16)
        junk = sb(gs, "junk", [128, D], BF16)
        sml = sb(gs, "sml", [128, 8], F32)
        Bx = Buf()
        xh_1 = sb(gs, "xh_1", [128, D], BF16)
        xhT_1 = sb(gs, "xhT_1", [128, 8, 128], BF16)
        junk_1 = junk
        sml_1 = sb(gs, "sml_1", [128, 8], F32)
        Bx_1 = Buf()
        FE = [(xh, xhT, junk, sml, Bx), (xh_1, xhT_1, junk_1, sml_1, Bx_1)]
        E = {"ot": [sb(gs, f"ot{i}", [128, 512], F32) for i in range(2)],
             "den": [sb(gs, f"den{i}", [64, 512], F32) for i in range(2)],
             "b": bufs(2)}

        def rstd_of(src_ap, n, jk, ssv, rs, R, Bt, psum=False):
            if psum:
                O.act(jk, src_ap, AF.Square, R, [Bt], accum_out=ssv)
            else:
                O.stt(jk, src_ap, 1.0, src_ap, ALU.mult, ALU.mult, R, [Bt], accum_out=ssv)
            O.ts('dve', ssv, ssv, 1.0 / n, EPS, ALU.mult, ALU.add, [Bt], [Bt])
            O.act(ssv, ssv, AF.Ln, [Bt], [Bt])
            O.act(rs, ssv, AF.Exp, [Bt], [Bt], scale=-0.5)

        def fe_a(src_dram_ap, slot, fs=0):
            xh_, xhT_, junk_, sml_, Bx_ = FE[fs]
            O.dma('sp', xt[slot][:], src_dram_ap, (), [xtb[slot]])
            rstd_of(xt[slot][:], D, junk_[:], sml_[:, 0:1], sml_[:, 1:2], [xtb[slot]], Bx_)

        def fe_b1(slot, fs=0):
            xh_, xhT_, junk_, sml_, Bx_ = FE[fs]
            O.act(xh_[:], xt[slot][:], AF.Copy, [xtb[slot], Bx_], [Bx_], scale=sml_[:, 1:2])

        def fe_b2(fs=0, pbank=0):
            xh_, xhT_, junk_, sml_, Bx_ = FE[fs]
            pT = ps[pbank][:].bitcast(BF16)
            for k in range(8):
                O.tr(pT[:, k * 128:(k + 1) * 128], xh_[:, k * 128:(k + 1) * 128], ident_b[:], [Bx_, Bc], [psb[pbank]])
            O.cp('dve' if fs else 'act', xhT_[:].rearrange("p k t -> p (k t)"), pT, [psb[pbank]], [Bx_])

        def front_end(src_dram_ap, slot, fs=0, pbank=0):
            xh_, xhT_, junk_, sml_, Bx_ = FE[fs]
            O.dma('sp', xt[slot][:], src_dram_ap, (), [xtb[slot]])
            rstd_of(xt[slot][:], D, junk_[:], sml_[:, 0:1], sml_[:, 1:2], [xtb[slot]], Bx_)
            O.act(xh_[:], xt[slot][:], AF.Copy, [xtb[slot], Bx_], [Bx_], scale=sml_[:, 1:2])
            pT = ps[pbank][:].bitcast(BF16)
            for k in range(8):
                O.tr(pT[:, k * 128:(k + 1) * 128], xh_[:, k * 128:(k + 1) * 128], ident_b[:], [Bx_, Bc], [psb[pbank]])
            O.cp('dve' if fs else 'act', xhT_[:].rearrange("p k t -> p (k t)"), pT, [psb[pbank]], [Bx_])

        sW1 = gs.enter_context(ExitStack())
        W1 = sb(sW1, "W1", [128, 8, 2176], BF16)
        bW1 = Buf()
        wuq = sb(sW1, "wuq", [128, 3, 768], BF16)
        wba = sb(sW1, "wba", [64, 8, D], BF16)
        Bw = Buf()
        w_in_v0 = A["w_in"].rearrange("(c p) n -> p c n", p=128)
        O.dma('pool', W1[:, :, 0:1152], w_in_v0[:, :, 0:1152], (), [bW1])
        O.dma('pool', W1[:, :, 1152:2176], w_in_v0[:, :, 1440:2464], (), [bW1])
        O.dma('pool', wuq[:], A["w_uq"].rearrange("(c p) n -> p c n", p=128), (), [Bw])
        O.dma('pool', wba[:], A["w_ba"].rearrange("(h p) n -> p h n", p=64), (), [Bw])
        with ExitStack() as st:
            ones_f = sb(st, "ones_f", [128, 128], F32)
            O.memset('dve', ones_f[:], 1.0, [Bc])
            ccol = sb(st, "ccol", [128, 8], F32)
            cact = sb(st, "cact", [128, 8], F32)
            crep = sb(st, "crep", [128, 8, 128], BF16)
            modB = sb(st, "modB", [128, 6 * D], F32)
            badaB = sb(st, "badaB", [128, 6 * D], F32)
            nmcol = sb(st, "nmcol", [128, 8], F32)
            nfB = sb(st, "nfB", [128, D], F32)
            wada = [sb(st, f"wada{i}", [128, 8, 512], BF16) for i in range(3)]
            wadab = bufs(3)
            sc1col = sb(st, "sc1col", [128, 8], F32)
            sh1f = sb(st, "sh1f", [128, 8], F32)
            B0 = Buf()
            O.dma('sp', ccol[:], A["c_col"], (), [B0])
            O.dma('sp', badaB[:], A["b_ada_b"], (), [B0])
            O.dma('sp', nmcol[:], A["norm_mix_col"], (), [B0])
            O.dma('sp', nfB[:], A["norm_ffn_b"], (), [B0])
            O.act(cact[:], ccol[:], AF.Silu, [B0], [B0])
            for k in range(8):
                O.act(crep[:, k, :], ones_f[:], AF.Copy, [B0, Bc], [B0], scale=cact[:, k:k + 1])
            wv = A["w_ada"].rearrange("(c p) n -> p c n", p=128)
            for n in range(12):
                sl = n % 3
                O.dma('pool', wada[sl][:], wv[:, :, n * 512:(n + 1) * 512], (), [wadab[sl]])
                pb = n % 2
                for k in range(8):
                    O.mm(ps[pb][:], crep[:, k, :], wada[sl][:, k, :], k == 0, k == 7, [B0, wadab[sl]], [psb[pb]])
                O.tt('dve', modB[:, n * 512:(n + 1) * 512], ps[pb][:], badaB[:, n * 512:(n + 1) * 512], ALU.add,
                     [psb[pb], B0], [B0])
            for which, dst in ((0, sh1f), (1, sc1col)):
                for c in range(8):
                    pb = 2 + (c % 2)
                    O.tr(ps[pb][:, 0:128], modB[:, which * D + c * 128: which * D + (c + 1) * 128], ident_f[:],
                         [B0, Bc], [psb[pb]])
                    O.cp('dve', dst[:, c:c + 1], ps[pb][:, 0:1], [psb[pb]], [B0])
            O.cp('dve', sh1col[:], sh1f[:], [B0], [Bmod])
            O.ts('dve', sc1col[:], sc1col[:], 1.0, None, ALU.add, None, [B0], [B0])
            O.tt('dve', G1col[:], sc1col[:], nmcol[:], ALU.mult, [B0], [Bmod])
            O.cp('dve', g1B[:], modB[:, 2 * D:3 * D], [B0], [Bmod])
            O.cp('dve', sh2B[:], modB[:, 3 * D:4 * D], [B0], [Bmod])
            O.ts('dve', G2B[:], modB[:, 4 * D:5 * D], 1.0, None, ALU.add, None, [B0], [Bmod])
            O.tt('dve', G2B[:], G2B[:], nfB[:], ALU.mult, [Bmod, B0], [Bmod])
            O.cp('dve', g2B[:], modB[:, 5 * D:6 * D], [B0], [Bmod])
            P.barrier()

        w_in_v = A["w_in"].rearrange("(c p) n -> p c n", p=128)

        def load_win_dma(wt, wb, col_ranges):
            off = 0
            for (a, b) in col_ranges:
                O.dma('pool', wt[:, :, off:off + (b - a)], w_in_v[:, :, a:b], (), [wb])
                off += b - a
            return off

        def load_win(wt, wb, brow, col_ranges, ncols=None):
            if ncols is None:
                ncols = load_win_dma(wt, wb, col_ranges)
            for n0 in range(0, ncols, 512):
                n1 = min(ncols, n0 + 512)
                for k in range(8):
                    O.mm(ps[7][0:1, 0:n1 - n0], sh1col[:, k:k + 1], wt[:, k, n0:n1], k == 0, k == 7,
                         [Bmod, wb], [psb[7]])
                O.cp('dve', brow[0:1, n0:n1], ps[7][0:1, 0:n1 - n0], [psb[7]], [wb])
            for k in range(8):
                O.act(wt[:, k, 0:ncols], wt[:, k, 0:ncols], AF.Copy, [Bmod, wb], [wb], scale=G1col[:, k:k + 1])

        def alloc_rope(st, pfx):
            T = {}
            for nm in ("ang", "u", "kf", "r", "m", "sin", "cos", "freq"):
                T[nm] = sb(st, pfx + nm, [128, 64], F32)
            T["posf"] = sb(st, pfx + "posf", [128, 4], F32)
            T["ki"] = sb(st, pfx + "ki", [128, 64], I32)
            return T

        def rope_tables(T, pos_i32_ap, Bt):
            O.cp('dve', T["posf"][:], pos_i32_ap, [Bt], [Bt])
            for j in range(4):
                O.ts('dve', T["ang"][:, j * 16:(j + 1) * 16], T["freq"][:, j * 16:(j + 1) * 16],
                     T["posf"][:, j:j + 1], None, ALU.mult, None, [Bt], [Bt])
            O.ts('dve', T["u"][:], T["ang"][:], 1.0 / TWO_PI, None, ALU.mult, None, [Bt], [Bt])
            O.cp('dve', T["ki"][:], T["u"][:], [Bt], [Bt])
            O.cp('dve', T["kf"][:], T["ki"][:], [Bt], [Bt])
            C1 = 6.28125
            C2 = TWO_PI - 6.28125
            O.stt(T["r"][:], T["kf"][:], -C1, T["ang"][:], ALU.mult, ALU.add, [Bt], [Bt])
            O.stt(T["r"][:], T["kf"][:], -C2, T["r"][:], ALU.mult, ALU.add, [Bt], [Bt])

            def wrap(x):
                O.ts('dve', T["m"][:], x, PI, -TWO_PI, ALU.is_gt, ALU.mult, [Bt], [Bt])
                O.tt('dve', x, x, T["m"][:], ALU.add, [Bt], [Bt])
                O.ts('dve', T["m"][:], x, -PI, TWO_PI, ALU.is_lt, ALU.mult, [Bt], [Bt])
                O.tt('dve', x, x, T["m"][:], ALU.add, [Bt], [Bt])
            wrap(T["r"][:])
            O.act(T["sin"][:], T["r"][:], AF.Sin, [Bt], [Bt])
            O.ts('dve', T["r"][:], T["r"][:], PI / 2, None, ALU.add, None, [Bt], [Bt])
            wrap(T["r"][:])
            O.act(T["cos"][:], T["r"][:], AF.Sin, [Bt], [Bt])

        def apply_rope(src, dst, nh, cosj, sinj, tmp, Rr, Bt, Wd):
            cb = cosj.unsqueeze(1).broadcast_to([128, nh, 16])
            sbb = sinj.unsqueeze(1).broadcast_to([128, nh, 16])
            t1 = src[:, :, 0:16]
            t2 = src[:, :, 16:32]
            a, b = tmp
            O.tt('dve', a, t1, cb, ALU.mult, Rr, [Bt])
            O.tt('dve', b, t2, sbb, ALU.mult, Rr, [Bt])
            O.tt('dve', dst[:, :, 0:16], a, b, ALU.subtract, [Bt], Wd)
            O.tt('dve', a, t2, cb, ALU.mult, Rr, [Bt])
            O.tt('dve', b, t1, sbb, ALU.mult, Rr, [Bt])
            O.tt('dve', dst[:, :, 16:32], a, b, ALU.add, [Bt], Wd)

        def attn_epilogue(pbank, pbuf, slot, dst_ap, extra_den, Wd):
            ot, den = E["ot"][slot], E["den"][slot]
            Bo = E["b"][slot]
            O.cp('act', ot[:], pbank[:], [pbuf], [Bo])
            O.dma('sp', den[0:64, :], ot[64:128, :], [Bo], [Bo])
            if extra_den is not None:
                O.tt('dve', den[0:64, :], den[0:64, :], extra_den, ALU.add, [Bo, Bc], [Bo])
            O.recip(den[0:64, :], den[0:64, :], [Bo], [Bo])
            if len(dst_ap.shape) == 3:
                hh = dst_ap.shape[1]
                O.tt('dve', dst_ap, ot[0:64, :].rearrange("p (h q) -> p h q", h=hh),
                     den[0:64, :].rearrange("p (h q) -> p h q", h=hh), ALU.mult, [Bo], Wd)
            else:
                O.tt('dve', dst_ap, ot[0:64, :], den[0:64, :], ALU.mult, [Bo], Wd)

        if phases >= 1:
          with ExitStack() as st:
            brow1 = sb(st, "brow1", [1, 2176], BF16)
            load_win(W1, bW1, brow1, None, ncols=2176)
            qncol = sb(st, "qncol", [128, 3], F32)
            O.dma('sp', qncol[:], A["q_norm_col"], (), [Bw])
            for c in range(3):
                O.act(wuq[:, c, :], wuq[:, c, :], AF.Copy, [Bw], [Bw], scale=qncol[:, c:c + 1])
            zeros_f = sb(st, "zeros_f", [128, 128], F32)
            O.memset('dve', zeros_f[:], 0.0, [Bc])
            swat = sb(st, "swat", [128, 2, 8, 128], F32)
            halo = sb(st, "halo", [128, 16], F32)
            sinksB = sb(st, "sinksB", [128, 8], F32)
            esink = sb(st, "esink", [64, 8, 128], F32)
            posq = sb(st, "posq", [128, 16], I32)
            RT = alloc_rope(st, "rq_")
            Bt = Buf()
            O.dma('sp', swat[:].rearrange("p a h q -> p (a h q)"), A["swa_tab"], (), [Bc])
            O.dma('sp', halo[:], A["halo_bias"], (), [Bc])
            O.dma('sp', sinksB[:], A["sinks_b"], (), [Bc])
            O.dma('sp', posq[:], A["posq"], (), [Bt])
            O.dma('sp', RT["freq"][:], A["freq_b"], (), [Bt])
            for h in range(8):
                O.act(esink[:, h, :], zeros_f[0:64, :], AF.Exp, [Bc], [Bc], bias=sinksB[0:64, h:h + 1])
            KTs = sb(st, "KTs", [128, 5, 128], BF16)
            VOs = sb(st, "VOs", [128, 5, 2, 128], BF16)
            QTs = sb(st, "QTs", [128, 4, 4, 128], BF16)
            siga = sb(st, "siga", [128, 4, D], BF16)
            OAT = sb(st, "OAT", [64, 8, 512], BF16)
            QTm = sb(st, "QTm", [96, 8, 512], BF16)
            zat = sb(st, "zat", [128, D], BF16)
            Bsb = Buf()
            Bzat = Buf()
            BQTm = Buf()
            O.memset('pool', VOs[:, :, :, 64:128], 1.0, [Bsb])
            qa_tm = sb(st, "qa_tm", [128, 512], BF16)
            kv_tm = sb(st, "kv_tm", [128, 128], BF16)
            cqn = sb(st, "cqn", [128, 384], BF16)
            cqnT = sb(st, "cqnT", [128, 3, 128], BF16)
            q_tm = sb(st, "q_tm", [128, 8, 96], BF16)
            qst = sb(st, "qst", [128, 768], F32)
            rtmp = [sb(st, f"rtmp{i}", [128, 8, 16], F32) for i in range(2)]
            stmp = sb(st, "stmp", [128, 512], F32)
            PT = [sb(st, f"PT{i}", [128, 512], BF16) for i in range(2)]
            PTb = bufs(2)
            Bq = Buf()
            pT6 = ps[6][:].bitcast(BF16)
            pT7 = ps[7][:].bitcast(BF16)
            for i in range(4):
                rope_tables(RT, posq[:, 4 * i:4 * i + 4], Bt)
                for r in range(5):
                    front_end(A["xq"][i, r * 128:(r + 1) * 128, :], r % 2)
                    if r == 0:
                        for k in range(8):
                            O.mm(ps[1][:, 0:256], xhT[:, k, :], W1[:, k, 512:768], k == 0, False, [Bx, bW1], [psb[1]])
                        O.mm(ps[1][:, 0:256], ones_b[0:1, :], brow1[0:1, 512:768], False, True, [Bc, bW1], [psb[1]])
                        kps, kbuf = ps[1], psb[1]
                    else:
                        for (pb, c0, c1) in ((1, 0, 512), (2, 512, 1024), (3, 1024, 1152)):
                            for k in range(8):
                                O.mm(ps[pb][:, 0:c1 - c0], xhT[:, k, :], W1[:, k, c0:c1], k == 0, False, [Bx, bW1], [psb[pb]])
                            O.mm(ps[pb][:, 0:c1 - c0], ones_b[0:1, :], brow1[0:1, c0:c1], False, True, [Bc, bW1], [psb[pb]])
                        for hf in range(2):
                            pb = 4 + hf
                            c0 = 1152 + hf * 512
                            for k in range(8):
                                O.mm(ps[pb][:], xhT[:, k, :], W1[:, k, c0:c0 + 512], k == 0, False, [Bx, bW1], [psb[pb]])
                            O.mm(ps[pb][:], ones_b[0:1, :], brow1[0:1, c0:c0 + 512], False, True, [Bc, bW1], [psb[pb]])
                            O.act(siga[:, r - 1, hf * 512:(hf + 1) * 512], ps[pb][:], AF.Sigmoid, [psb[pb]], [Bsb])
                        kps, kbuf = ps[2], psb[2]
                    O.cp('dve', kv_tm[:], kps[:, 0:128], [kbuf], [Bq])
                    O.cp('dve', VOs[:, r, :, 0:64], kps[:, 128:256].rearrange("p (g d) -> p g d", g=2), [kbuf], [Bsb])
                    O.tr(pT6[:, 0:128], kv_tm[:], ident_b[:], [Bq, Bc], [psb[6]])
                    O.cp('dve', KTs[:, r, :], pT6[:, 0:128], [psb[6]], [Bsb])
                    if r == 0:
                        continue
                    O.cp('act', qa_tm[:].rearrange("p (a g d) -> p g a d", a=4, g=2),
                         ps[1][:].rearrange("p (g a d) -> p g a d", g=2, a=4), [psb[1]], [Bq])
                    for a in range(4):
                        O.tr(pT6[:, 128 + a * 128:128 + (a + 1) * 128], qa_tm[:, a * 128:(a + 1) * 128], ident_b[:], [Bq, Bc], [psb[6]])
                    O.cp('act', QTs[:, r - 1, :, :].rearrange("p a q -> p (a q)"), pT6[:, 128:640], [psb[6]], [Bsb])
                    O.act(junk[:, 0:256], ps[2][:, 256:512], AF.Square, [psb[2]], [Bq], accum_out=sml[:, 2:3])
                    O.act(junk[:, 256:384], ps[3][:, 0:128], AF.Square, [psb[3]], [Bq], accum_out=sml[:, 3:4])
                    O.tt('dve', sml[:, 2:3], sml[:, 2:3], sml[:, 3:4], ALU.add, [Bq], [Bq])
                    O.ts('dve', sml[:, 2:3], sml[:, 2:3], 1.0 / 384, EPS, ALU.mult, ALU.add, [Bq], [Bq])
                    O.act(sml[:, 2:3], sml[:, 2:3], AF.Ln, [Bq], [Bq])
                    O.act(sml[:, 4:5], sml[:, 2:3], AF.Exp, [Bq], [Bq], scale=-0.5)
                    O.act(cqn[:, 0:256], ps[2][:, 256:512], AF.Copy, [psb[2], Bq], [Bq], scale=sml[:, 4:5])
                    O.act(cqn[:, 256:384], ps[3][:, 0:128], AF.Copy, [psb[3], Bq], [Bq], scale=sml[:, 4:5])
                    for c in range(3):
                        O.tr(pT7[:, c * 128:(c + 1) * 128], cqn[:, c * 128:(c + 1) * 128], ident_b[:], [Bq, Bc], [psb[7]])
                    O.cp('act', cqnT[:].rearrange("p c t -> p (c t)"), pT7[:, 0:384], [psb[7]], [Bq])
                    for (pb, c0, c1) in ((2, 0, 512), (3, 512, 768)):
                        for c in range(3):
                            O.mm(ps[pb][:, 0:c1 - c0], cqnT[:, c, :], wuq[:, c, c0:c1], c == 0, c == 2, [Bq, Bw], [psb[pb]])
                    O.cp('act', qst[:, 0:512], ps[2][:], [psb[2]], [Bq])
                    O.cp('act', qst[:, 512:768], ps[3][:, 0:256], [psb[3]], [Bq])
                    qs3 = qst[:].rearrange("p (h d) -> p h d", h=8)
                    O.cp('pool', q_tm[:, :, 0:64], qs3[:, :, 0:64], [Bq], [Bq])
                    j = r - 1
                    apply_rope(qs3[:, :, 64:96], q_tm[:, :, 64:96], 8, RT["cos"][:, j * 16:(j + 1) * 16],
                               RT["sin"][:, j * 16:(j + 1) * 16], (rtmp[0][:], rtmp[1][:]), [Bq, Bt], Bq, [Bq])
                    for h in range(8):
                        O.tr(pT7[0:96, h * 128:(h + 1) * 128], q_tm[:, h, :], ident_b[:], [Bq, Bc], [psb[7]])
                    O.cp('act', QTm[:, :, j * 128:(j + 1) * 128], pT7[0:96, :].rearrange("p (h t) -> p h t", h=8),
                         [psb[7]], [BQTm])
                O.dma('sp', qT_d[:, :, i * 512:(i + 1) * 512].rearrange("h p t -> p h t"), QTm[:], [BQTm], [BqT_d])
                for r in range(1, 5):
                    ti = 4 * i + (r - 1)
                    for g in range(2):
                        pacc = 4 + g
                        for typ, kt in ((0, r - 1), (1, r)):
                            pb = 1 + typ
                            O.mm(ps[pb][:], KTs[64 * g:64 * g + 64, kt, :],
                                 QTs[64 * g:64 * g + 64, r - 1, :, :].rearrange("p a q -> p (a q)"),
                                 True, True, [Bsb], [psb[pb]])
                            O.stt(stmp[:], ps[pb][:], 0.125,
                                  swat[:, typ, 4 * g:4 * g + 4, :].rearrange("p h q -> p (h q)"),
                                  ALU.mult, ALU.add, [psb[pb], Bc], [Bq])
                            sl = typ
                            if typ == 0:
                                O.act(PT[sl][:], stmp[:], AF.Exp, [Bq, Bc], [PTb[sl]], bias=halo[:, ti:ti + 1])
                            else:
                                O.act(PT[sl][:], stmp[:], AF.Exp, [Bq], [PTb[sl]])
                            O.mm(ps[pacc][:], VOs[:, kt, g, :], PT[sl][:], typ == 0, typ == 1, [Bsb, PTb[sl]], [psb[pacc]])
                        attn_epilogue(ps[pacc], psb[pacc], g,
                                      OAT[:, 4 * g:4 * g + 4, (r - 1) * 128:r * 128],
                                      esink[:, 4 * g:4 * g + 4, :].rearrange("p h q -> p (h q)"), [Bsb])
                    for hf in range(2):
                        pb = 6 + hf
                        for h in range(8):
                            O.mm(ps[pb][:], OAT[:, h, (r - 1) * 128:r * 128], wba[:, h, hf * 512:(hf + 1) * 512],
                                 h == 0, h == 7, [Bsb, Bw], [psb[pb]])
                        O.tt('dve', zat[:, hf * 512:(hf + 1) * 512], ps[pb][:], siga[:, r - 1, hf * 512:(hf + 1) * 512],
                             ALU.mult, [psb[pb], Bsb], [Bzat])
                    O.dma('sp', za_d[ti], zat[:], [Bzat], [Bza_d])
            P.barrier()

        sW1.close()

        if phases >= 2:
          with ExitStack() as st:
            W2 = sb(st, "W2", [128, 8, 288], BF16)
            bW2 = Buf()
            brow2 = sb(st, "brow2", [1, 288], BF16)
            load_win(W2, bW2, brow2, [(1152, 1440)])
            posall = sb(st, "posall", [128, 64], I32)
            RT = alloc_rope(st, "rk_")
            Bt = Buf()
            O.dma('sp', posall[:], A["posall"], (), [Bt])
            O.dma('sp', RT["freq"][:], A["freq_b"], (), [Bt])
            ckvn = sb(st, "ckvn", [128, 256], BF16)
            kr_tm = sb(st, "kr_tm", [128, 1, 32], BF16)
            ktmp = [sb(st, f"ktmp{i}", [128, 1, 16], F32) for i in range(2)]
            ckvT_st = sb(st, "ckvT_st", [128, 2, 512], BF16)
            krT_st = sb(st, "krT_st", [32, 512], BF16)
            Bk = Buf()
            Bst = Buf()
            pT6 = ps[6][:].bitcast(BF16)
            pT7 = ps[7][:].bitcast(BF16)
            ckvn2 = [ckvn, sb(st, "ckvn_1", [128, 256], BF16)]
            kr_tm2 = [kr_tm, sb(st, "kr_tm_1", [128, 1, 32], BF16)]
            Bk2 = [Bk, Buf()]
            sml2 = [sml, sml_1]

            def xsrc(t):
                return A["xall"][t * 128:(t + 1) * 128, :]

            def d2(t):
                grp, j = t // 4, t % 4
                f = t % 2
                for c in range(2):
                    O.tr(pT6[:, (2 * j + c) * 128:(2 * j + c + 1) * 128], ckvn2[f][:, c * 128:(c + 1) * 128], ident_b[:],
                         [Bk2[f], Bc], [psb[6]])
                O.tr(pT7[0:32, j * 128:(j + 1) * 128], kr_tm2[f][:, 0, :], ident_b[:], [Bk2[f], Bc], [psb[7]])
                if j == 3:
                    O.cp('act', ckvT_st[:].rearrange("p c (j t) -> p j c t", j=4),
                         pT6[:, 0:1024].rearrange("p (j c t) -> p j c t", j=4, c=2), [psb[6]], [Bst])
                    O.cp('dve', krT_st[:], pT7[0:32, 0:512], [psb[7]], [Bst])
                    O.dma('sp', ckvT_d[:, :, grp * 512:(grp + 1) * 512].rearrange("c p t -> p c t"), ckvT_st[:], [Bst], [Bckv_d])
                    O.dma('sp', krT_d[:, grp * 512:(grp + 1) * 512], krT_st[:], [Bst], [Bkr_d])
            fe_a(xsrc(0), 0, 0)
            fe_a(xsrc(1), 1, 1)
            fe_b1(0, 0)
            fe_b2(0, 0)
            for t in range(64):
                grp, j = t // 4, t % 4
                f = t % 2
                xhT_c, Bx_c = FE[f][1], FE[f][4]
                if j == 0:
                    rope_tables(RT, posall[:, 4 * grp:4 * grp + 4], Bt)
                if t + 1 < 64:
                    fe_b1((t + 1) % 2, (t + 1) % 2)
                pp = 1 if f == 0 else 3
                for k in range(8):
                    O.mm(ps[pp][:, 0:288], xhT_c[:, k, :], W2[:, k, :], k == 0, False, [Bx_c, bW2], [psb[pp]])
                O.mm(ps[pp][:, 0:288], ones_b[0:1, :], brow2[0:1, :], False, True, [Bc, bW2], [psb[pp]])
                if t + 1 < 64:
                    fe_b2((t + 1) % 2, 0 if (t + 1) % 2 == 0 else 2)
                if t + 2 < 64:
                    fe_a(xsrc(t + 2), (t + 2) % 2, (t + 2) % 2)
                rstd_of(ps[pp][:, 0:256], 256, junk[:, 0:256], sml2[f][:, 2:3], sml2[f][:, 4:5], [psb[pp]], Bk2[f], psum=True)
                O.act(ckvn2[f][:], ps[pp][:, 0:256], AF.Copy, [psb[pp], Bk2[f]], [Bk2[f]], scale=sml2[f][:, 4:5])
                apply_rope(ps[pp][:, 256:288].rearrange("p (h d) -> p h d", h=1), kr_tm2[f][:], 1,
                           RT["cos"][:, j * 16:(j + 1) * 16], RT["sin"][:, j * 16:(j + 1) * 16],
                           (ktmp[0][:], ktmp[1][:]), [psb[pp], Bt], Bk2[f], [Bk2[f]])
                if t >= 1:
                    d2(t - 1)
            d2(63)
            P.barrier()

        if phases >= 3:
          with ExitStack() as st:
            ckvT = sb(st, "ckvT", [128, 2, S], BF16)
            KT = sb(st, "KT", [128, S], BF16)
            VO = sb(st, "VO", [128, 64, 128], BF16)
            QT = [sb(st, f"QT{i}", [128, 2048], BF16) for i in range(2)]
            QTb = bufs(2)
            OBs = sb(st, "OBs", [64, 2048], BF16)
            wuk = sb(st, "wuk", [128, 2, 512], BF16)
            wuv = sb(st, "wuv", [128, 2, 512], BF16)
            kvn = sb(st, "kvn", [128, 2], F32)
            qidxB = sb(st, "qidxB", [128, 2048], F32)
            kidx = sb(st, "kidx", [128, 64], F32)
            EP = [sb(st, f"EP{i}", [128, 512], BF16) for i in range(6)]
            EPb = bufs(6)
            Bl, BKT, BVO, Bw3, BOBs = bufs(5)
            for c in range(2):
                O.dma('sp', ckvT[:, c, :], ckvT_d[c], [Bckv_d], [Bl])
            O.memset('pool', KT[96:128, :], 0.0, [BKT])
            O.dma('sp', KT[64:96, :], krT_d, [Bkr_d], [BKT])
            for qq in range(2):
                O.memset('pool', QT[qq][96:128, :], 0.0, [QTb[qq]])
            O.dma('sp', qidxB[:], A["qidx"], (), [Bc])
            O.dma('sp', kidx[:], A["kidx"], (), [Bc])
            O.dma('pool', wuk[:], A["w_uk"].rearrange("(c p) n -> p c n", p=128), (), [Bw3])
            O.dma('pool', wuv[:], A["w_uv"].rearrange("(c p) n -> p c n", p=128), (), [Bw3])
            O.dma('sp', kvn[:], A["kv_norm_col"], (), [Bw3])
            for c in range(2):
                O.act(wuk[:, c, :], wuk[:, c, :], AF.Copy, [Bw3], [Bw3], scale=kvn[:, c:c + 1])
                O.act(wuv[:, c, :], wuv[:, c, :], AF.Copy, [Bw3], [Bw3], scale=kvn[:, c:c + 1])
            O.memset('pool', VO[:, :, 64:128], 1.0, [BVO])
            tcount = 0
            for h in range(8):
                qs = h % 2
                O.dma('sp', QT[qs][0:96, :], qT_d[h], [BqT_d], [QTb[qs]])
                for n in range(16):
                    pb = 1 + n % 2
                    for c in range(2):
                        O.mm(ps[pb][0:64, :], wuk[:, c, h * 64:(h + 1) * 64], ckvT[:, c, n * 512:(n + 1) * 512],
                             c == 0, c == 1, [Bw3, Bl], [psb[pb]])
                    O.cp('act' if n % 2 == 0 else 'dve', KT[0:64, n * 512:(n + 1) * 512], ps[pb][0:64, :], [psb[pb]], [BKT])
                for n in range(8):
                    pb = 3
                    for kb8 in range(8):
                        kb = n * 8 + kb8
                        for c in range(2):
                            O.mm(ps[pb][:, kb8 * 64:(kb8 + 1) * 64], ckvT[:, c, kb * 128:(kb + 1) * 128],
                                 wuv[:, c, h * 64:(h + 1) * 64], c == 0, c == 1, [Bw3, Bl], [psb[pb]])
                    O.cp('dve' if n % 2 == 0 else 'act', VO[:, n * 8:(n + 1) * 8, 0:64],
                         ps[pb][:].rearrange("p (b d) -> p b d", b=8), [psb[pb]], [BVO])
                tiles = [(i, kb) for i in range(4) for kb in range(16 * (i + 1))]
                slots = {}
                LA = 4
                for n in range(len(tiles) + LA):
                    if n < len(tiles):
                        i, kb = tiles[n]
                        pbS = tcount % 6
                        slots[n] = pbS
                        tcount += 1
                        O.mm(ps[pbS][:], KT[:, kb * 128:(kb + 1) * 128], QT[qs][:, i * 512:(i + 1) * 512],
                             True, True, [BKT, QTb[qs]], [psb[pbS]])
                        O.act(EP[pbS][:], ps[pbS][:], AF.Exp, [psb[pbS]], [EPb[pbS]], scale=SCALE_MLA)
                        if kb >= 16 * i:
                            O.stt(EP[pbS][:], qidxB[:, i * 512:(i + 1) * 512], kidx[:, kb:kb + 1], EP[pbS][:],
                                  ALU.is_ge, ALU.mult, [Bc, EPb[pbS]], [EPb[pbS]])
                    m = n - LA
                    if m >= 0:
                        i, kb = tiles[m]
                        sl = slots[m]
                        nkb = 16 * (i + 1)
                        pacc = 6 + (i % 2)
                        O.mm(ps[pacc][:], VO[:, kb, :], EP[sl][:], kb == 0, kb == nkb - 1, [BVO, EPb[sl]], [psb[pacc]])
                        if kb == nkb - 1:
                            attn_epilogue(ps[pacc], psb[pacc], i % 2, OBs[:, i * 512:(i + 1) * 512], None, [BOBs])
                O.dma('sp', obT_d[h], OBs[:], [BOBs], [Bob_d])
            P.barrier()

        if phases >= 4:
          with ExitStack() as sX:
            x1 = sb(sX, "x1", [128, 16, D], F32)
            Bx1 = bufs(16)
            with ExitStack() as st:
                Wg = sb(st, "Wg", [128, 8, D], BF16)
                bWg = Buf()
                browg = sb(st, "browg", [1, D], BF16)
                load_win(Wg, bWg, browg, [(2464, 3488)])
                wbb = sb(st, "wbb", [64, 8, D], BF16)
                wout = sb(st, "wout", [128, 8, D], BF16)
                Bw4 = Buf()
                O.dma('pool', wbb[:], A["w_bb"].rearrange("(h p) n -> p h n", p=64), (), [Bw4])
                O.dma('pool', wout[:], A["w_out"].rearrange("(c p) n -> p c n", p=128), (), [Bw4])
                for k in range(8):
                    O.tt('dve', wout[:, k, :], wout[:, k, :], g1B[:], ALU.mult, [Bw4, Bmod], [Bw4])
                OBt = sb(st, "OBt", [64, 8, 128], BF16)
                zat = sb(st, "zat4", [128, D], BF16)
                sigb = sb(st, "sigb", [128, D], BF16)
                mixed = sb(st, "mixed", [128, D], BF16)
                mixT = sb(st, "mixT", [128, 8, 128], BF16)
                tmpf = sb(st, "tmpf", [128, D], F32)
                Bl4, Bm4 = bufs(2)
                pT6 = ps[6][:].bitcast(BF16)
                for ti in range(16):
                    i, r = ti // 4, ti % 4 + 1
                    front_end(A["xq"][i, r * 128:(r + 1) * 128, :], ti % 2)
                    O.dma('sp', OBt[:], obT_d[:, :, ti * 128:(ti + 1) * 128].rearrange("h p t -> p h t"), [Bob_d], [Bl4])
                    O.dma('sp', zat[:], za_d[ti], [Bza_d], [Bl4])
                    for hf in range(2):
                        pb = 1 + hf
                        c0 = hf * 512
                        for k in range(8):
                            O.mm(ps[pb][:], xhT[:, k, :], Wg[:, k, c0:c0 + 512], k == 0, False, [Bx, bWg], [psb[pb]])
                        O.mm(ps[pb][:], ones_b[0:1, :], browg[0:1, c0:c0 + 512], False, True, [Bc, bWg], [psb[pb]])
                        O.act(sigb[:, c0:c0 + 512], ps[pb][:], AF.Sigmoid, [psb[pb]], [Bm4])
                        pb2 = 3 + hf
                        for h in range(8):
                            O.mm(ps[pb2][:], OBt[:, h, :], wbb[:, h, c0:c0 + 512], h == 0, h == 7, [Bl4, Bw4], [psb[pb2]])
                        O.tt('dve', tmpf[:, c0:c0 + 512], ps[pb2][:], sigb[:, c0:c0 + 512], ALU.mult, [psb[pb2], Bm4], [Bm4])
                    O.tt('dve', mixed[:], tmpf[:], zat[:], ALU.add, [Bm4, Bl4], [Bm4])
                    for k in range(8):
                        O.tr(pT6[:, k * 128:(k + 1) * 128], mixed[:, k * 128:(k + 1) * 128], ident_b[:], [Bm4, Bc], [psb[6]])
                    O.cp('act', mixT[:].rearrange("p k t -> p (k t)"), pT6, [psb[6]], [Bm4])
                    for hf in range(2):
                        pb = 1 + hf
                        c0 = hf * 512
                        for k in range(8):
                            O.mm(ps[pb][:], mixT[:, k, :], wout[:, k, c0:c0 + 512], k == 0, k == 7, [Bm4, Bw4], [psb[pb]])
                        O.tt('dve', x1[:, ti, c0:c0 + 512], ps[pb][:], xt[ti % 2][:, c0:c0 + 512], ALU.add,
                             [psb[pb], xtb[ti % 2]], [Bx1[ti]])
                P.barrier()

            h2 = sb(sX, "h2", [128, 16, D], BF16)
            Bh2 = bufs(16)
            gwb = sb(sX, "gwb", [128, 16, 32], BF16)
            posm = sb(sX, "posm", [128, 16, 32], F32)
            maskb = sb(sX, "maskb", [128, 16, 32], BF16)
            Brt = Buf()
            with ExitStack() as st:
                wr = sb(st, "wr", [128, 8, 32], F32)
                brB = sb(st, "brB", [128, 32], F32)
                ut = sb(st, "ut", [128, 128], F32)
                utb = sb(st, "utb", [128, 128], BF16)
                Bw4 = Buf()
                O.dma('sp', wr[:], A["w_router"].rearrange("(c p) n -> p c n", p=128), (), [Bw4])
                O.dma('sp', brB[:], A["b_router_b"], (), [Bw4])
                O.dma('sp', ut[:], A["ut"], (), [Bw4])
                O.cp('dve', utb[:], ut[:], [Bw4], [Bw4])
                tmpf = sb(st, "tmpf2", [128, D], F32)
                b2all = sb(st, "b2all", [32, D], BF16)
                gwT = sb(st, "gwT", [32, 128], BF16)
                Bb2 = Buf()
                O.dma('pool', b2all[:], A["b_moe2"], (), [Bw4])
                h2f = sb(st, "h2f", [128, D], F32)
                h2fT = sb(st, "h2fT", [128, 8, 128], F32)
                lg = sb(st, "lg", [128, 32], F32)
                ex = sb(st, "ex", [128, 32], F32)
                mk = sb(st, "mk", [128, 32], F32)
                m8 = sb(st, "m8", [128, 8], F32)
                s4 = sb(st, "s4", [128, 8], F32)
                Bm4, Br4 = bufs(2)
                for ti in range(16):
                    rstd_of(x1[:, ti, :], D, junk[:], sml[:, 2:3], sml[:, 4:5], [Bx1[ti]], Br4)
                    O.stt(tmpf[:], x1[:, ti, :], sml[:, 4:5], G2B[:], ALU.mult, ALU.mult, [Bx1[ti], Br4, Bmod, Bm4], [Bm4])
                    O.tt('dve', h2f[:], tmpf[:], sh2B[:], ALU.add, [Bm4, Bmod], [Br4])
                    O.cp('act', h2[:, ti, :], h2f[:], [Br4], [Bh2[ti]])
                    for half in range(2):
                        pb = 3 + half
                        for k4 in range(4):
                            k = half * 4 + k4
                            O.tr(ps[pb][:, k4 * 128:(k4 + 1) * 128], h2f[:, k * 128:(k + 1) * 128], ident_f[:], [Br4, Bc], [psb[pb]])
                        O.cp('act' if half == 0 else 'dve', h2fT[:, half * 4:(half + 1) * 4, :].rearrange("p k t -> p (k t)"),
                             ps[pb][:], [psb[pb]], [Br4])
                    for k in range(8):
                        O.mm(ps[5][:, 0:32], h2fT[:, k, :], wr[:, k, :], k == 0, k == 7, [Br4, Bw4], [psb[5]])
                    O.tt('dve', lg[:], ps[5][:, 0:32], brB[:], ALU.add, [psb[5], Bw4], [Br4])
                    O.max8(m8[:], lg[:], [Br4], [Br4])
                    O.ts('dve', mk[:], lg[:], m8[:, 3:4], None, ALU.is_ge, None, [Br4], [Br4])
                    O.ts('dve', s4[:, 0:1], m8[:, 0:1], -1.0, None, ALU.mult, None, [Br4], [Br4])
                    O.act(ex[:], lg[:], AF.Exp, [Br4], [Br4], bias=s4[:, 0:1])
                    O.stt(ex[:], ex[:], 1.0, mk[:], ALU.mult, ALU.mult, [Br4], [Br4], accum_out=s4[:, 1:2])
                    O.recip(s4[:, 2:3], s4[:, 1:2], [Br4], [Br4])
                    O.ts('dve', gwb[:, ti, :], ex[:], s4[:, 2:3], None, ALU.mult, None, [Br4], [Brt])
                    O.cp('dve', maskb[:, ti, :], mk[:], [Br4], [Brt])
                    g0 = (ti // 4) * 4
                    for a in range(g0, ti + 1):
                        O.mm(ps[5][:, 32:64], (ones_b[:] if a < ti else utb[:]), maskb[:, a, :], a == g0, a == ti,
                             [Bc, Bw4, Brt], [psb[5]])
                    O.stt(ex[:], ps[5][:, 32:64], 1.0, mk[:], ALU.add, ALU.mult, [psb[5], Br4], [Br4])
                    O.ts('dve', posm[:, ti, :], ex[:], -1.0, None, ALU.add, None, [Br4], [Brt])
                    pT7b = ps[7][:].bitcast(BF16)
                    O.tr(pT7b[0:32, 0:128], gwb[:, ti, :], ident_b[:], [Brt, Bc], [psb[7]])
                    O.cp('act', gwT[:], pT7b[0:32, 0:128], [psb[7]], [Bb2])
                    for hf in range(2):
                        pb = 1 + hf
                        O.mm(ps[pb][:], gwT[:], b2all[:, hf * 512:(hf + 1) * 512], True, True, [Bb2, Bw4], [psb[pb]])
                        O.tt('dve', tmpf[:, hf * 512:(hf + 1) * 512], ps[pb][:], g2B[:, hf * 512:(hf + 1) * 512], ALU.mult,
                             [psb[pb], Bmod, Bm4, Br4], [Bm4])
                        O.tt('dve', x1[:, ti, hf * 512:(hf + 1) * 512], x1[:, ti, hf * 512:(hf + 1) * 512],
                             tmpf[:, hf * 512:(hf + 1) * 512], ALU.add, [Bm4, Bx1[ti], Bh2[ti]], [Bx1[ti]])
                P.barrier()

            with ExitStack() as st:
                ring = [sb(st, f"ring{i}", [128, 8, 512], BF16) for i in range(6)]
                ringb = bufs(6)
                iota = sh2B[:, 0:256]
                Sel = g1B[:].bitcast(BF16).rearrange("p (a j) -> p a j", a=8)
                SelT = G2B[:].bitcast(BF16).rearrange("p (g t) -> p g t", g=4)
                xg = sb(st, "xg", [128, 8, 512], BF16)
                actT = sb(st, "actT", [128, 8, 512], BF16)
                ysb3 = xh_1
                ysb = [xh[:], junk[:], xhT[:].rearrange("p k t -> p (k t)"), ysb3[:]]
                _al = xhT_1[:].rearrange("p k t -> p (k t)").bitcast(F32)
                wj = _al[:, 0:4]
                b1c = [_al[:, 16:32], _al[:, 32:48]]
                b2r = [E["den"][0][0:1, :].bitcast(BF16), E["den"][1][0:1, :].bitcast(BF16)]
                bb = bufs(2)
                SW = [(E["ot"][0][:], E["ot"][1][:], xt[0][:, 0:512]), (xt[0][:, 512:1024], xt[1][:, 0:512], xt[1][:, 512:1024])]
                Bsw2 = bufs(2)
                BSel, BSelT, Bxg, Bact, By, Bwj, Bsw = bufs(7)
                O.dma('sp', iota, A["iota_j"], (), [Bc])
                w1v = A["w_moe1"].rearrange("e (c p) n -> e p c n", p=128)
                w2v = A["w_moe2"].rearrange("e (c p) n -> e p c n", p=128)
                rr = 0
                pT6 = ps[6][:].bitcast(BF16)

                def load_piece(sl, src):
                    O.dma('pool', ring[sl][:, 0:4, :], src[:, 0:4, :], (), [ringb[sl]])
                    O.dma('pool', ring[sl][:, 4:8, :], src[:, 4:8, :], (), [ringb[sl]])
                wj2 = [_al[:, 0:4], _al[:, 8:12]]
                Bwj2 = bufs(2)
                G = 2 * n_experts
                wslots = {}

                def selb(g):
                    e, half = g // 2, g % 2
                    for a8 in range(8):
                        a = half * 8 + a8
                        O.ts('dve', Sel[:, a8, :], iota, posm[:, a, e:e + 1], None, ALU.is_equal, None, [Bc, Brt], [BSel])

                def wjg(g):
                    e, half = g // 2, g % 2
                    for gi2 in range(2):
                        for jc in range(2):
                            ch = gi2 * 2 + jc
                            for a4 in range(4):
                                a8 = gi2 * 4 + a4
                                a = half * 8 + a8
                                O.mm(ps[7][:, ch:ch + 1], Sel[:, a8, jc * 128:(jc + 1) * 128], gwb[:, a, e:e + 1],
                                     a4 == 0, a4 == 3, [BSel, Brt], [psb[7]])
                    O.cp('act', wj2[g % 2], ps[7][:, 0:4], [psb[7]], [Bwj2[g % 2]])
                    for c in range(8):
                        pb = (0, 1, 6, 7)[c % 4]
                        for gi2 in range(2):
                            for a4 in range(4):
                                a8 = gi2 * 4 + a4
                                a = half * 8 + a8
                                O.mm(ps[pb][:, gi2 * 256:(gi2 + 1) * 256], h2[:, a, c * 128:(c + 1) * 128], Sel[:, a8, :],
                                     a4 == 0, a4 == 3, [Bh2[a], BSel], [psb[pb]])
                        O.cp('act' if c % 2 == 0 else 'dve', xg[:, c, :], ps[pb][:], [psb[pb]], [Bxg])

                def selT(g):
                    for gi2 in range(2):
                        for jc in range(2):
                            ch = gi2 * 2 + jc
                            for a4 in range(4):
                                O.tr(pT6[:, a4 * 128:(a4 + 1) * 128], Sel[:, gi2 * 4 + a4, jc * 128:(jc + 1) * 128], ident_b[:],
                                     [BSel, Bc], [psb[6]])
                            O.cp('act', SelT[:, ch, :], pT6[:, 0:512], [psb[6]], [BSelT])

                def loads_A(e):
                    es = e % 2
                    load_piece(0, w1v[e][:, :, 0:512])
                    load_piece(1, w1v[e][:, :, 1024:1536])
                    O.dma('sp', b1c[es], A["b1c"][e], (), [bb[es]])

                def loads_B(e):
                    load_piece(2, w1v[e][:, :, 512:1024])
                    load_piece(3, w1v[e][:, :, 1536:2048])

                def loads_2(e, hf):
                    load_piece(4 + hf, w2v[e][:, :, hf * 512:(hf + 1) * 512])

                loads_A(0)
                loads_B(0)
                loads_2(0, 0)
                loads_2(0, 1)
                selb(0)
                wjg(0)
                selT(0)
                for g in range(G):
                    e, half = g // 2, g % 2
                    es = e % 2
                    wj = wj2[g % 2]
                    Bwj = Bwj2[g % 2]
                    if g + 1 < G:
                        selb(g + 1)
                    for m in range(8):
                        pg, pl = (2, 3) if m % 2 == 0 else (4, 5)
                        gt, sg, lr = SW[m % 2]
                        s_g, s_l = (0, 1) if m < 4 else (2, 3)
                        mc = (m % 4) * 128
                        for c in range(8):
                            O.mm(ps[pg][:], ring[s_g][:, c, mc:mc + 128], xg[:, c, :], c == 0, c == 7,
                                 [ringb[s_g], Bxg], [psb[pg]])
                        for c in range(8):
                            O.mm(ps[pl][:], ring[s_l][:, c, mc:mc + 128], xg[:, c, :], c == 0, c == 7,
                                 [ringb[s_l], Bxg], [psb[pl]])
                        Bs_ = Bsw2[m % 2]
                        O.ts('dve', gt, ps[pg][:], b1c[es][:, m:m + 1], 7.0, ALU.add, ALU.min, [psb[pg], bb[es]], [Bs_])
                        O.act(lr, ps[pl][:], AF.Identity, [psb[pl], bb[es]], [Bs_], bias=b1c[es][:, 8 + m:9 + m])
                        O.act(sg, gt, AF.Sigmoid, [Bs_], [Bs_], scale=1.702)
                        O.ts('dve', lr, lr, 7.0, -7.0, ALU.min, ALU.max, [Bs_], [Bs_])
                        O.tt('dve', gt, gt, sg, ALU.mult, [Bs_], [Bs_])
                        O.stt(actT[:, m, :], lr, 1.0, gt, ALU.add, ALU.mult, [Bs_], [Bact])
                        if m == 3 and half == 1 and e + 1 < n_experts:
                            loads_A(e + 1)
                    if half == 1 and e + 1 < n_experts:
                        loads_B(e + 1)
                    if g + 1 < G:
                        wjg(g + 1)
                    for hf in range(2):
                        for ch in range(4):
                            pb = 6 + ch % 2
                            for m in range(8):
                                O.mm(ps[pb][:], actT[:, m, ch * 128:(ch + 1) * 128], ring[4 + hf][:, m, :],
                                     m == 0, m == 7, [Bact, ringb[4 + hf]], [psb[pb]])
                            O.stt(ysb[ch][:, hf * 512:(hf + 1) * 512], ps[pb][:], wj[:, ch:ch + 1], g2B[:, hf * 512:(hf + 1) * 512],
                                  ALU.mult, ALU.mult, [psb[pb], Bwj, Bmod], [By])
                        if half == 1 and e + 1 < n_experts:
                            loads_2(e + 1, hf)
                    for a8 in range(8):
                        a = half * 8 + a8
                        gi2, a4 = a8 // 4, a8 % 4
                        for hf in range(2):
                            pb = (a8 * 2 + hf) % 6
                            for jc in range(2):
                                ch = gi2 * 2 + jc
                                O.mm(ps[pb][:], SelT[:, ch, a4 * 128:(a4 + 1) * 128], ysb[ch][:, hf * 512:(hf + 1) * 512],
                                     jc == 0, jc == 1, [BSelT, By], [psb[pb]])
                            O.tt('dve', x1[:, a, hf * 512:(hf + 1) * 512], x1[:, a, hf * 512:(hf + 1) * 512], ps[pb][:],
                                 ALU.add, [psb[pb], Bx1[a]], [Bx1[a]])
                    if g + 1 < G:
                        selT(g + 1)
                P.barrier()

            with ExitStack() as st:
                fnB = sb(st, "fnB", [128, D], F32)
                ob = [sb(st, f"ob{i}", [128, D], F32) for i in range(2)]
                obb = bufs(2)
                Bf = Buf()
                O.dma('sp', fnB[:], A["final_norm_b"], (), [Bf])
                outs = []
                for ti in range(16):
                    sl = ti % 2
                    rstd_of(x1[:, ti, :], D, junk[:], sml[:, 2:3], sml[:, 4:5], [Bx1[ti]], Bf)
                    O.stt(ob[sl][:], x1[:, ti, :], sml[:, 4:5], fnB[:], ALU.mult, ALU.mult, [Bx1[ti], Bf], [obb[sl]])
                    outs.append(O.dma('sp', out_d[ti * 128:(ti + 1) * 128, :], ob[sl][:], [obb[sl]], ()))
                P.wait_all('sp', outs)
        P.barrier()
        P.finish()
    return nc


def _host_inputs(inputs):
    f32 = np.float32
    x = np.asarray(inputs["x"], f32)
    c = np.asarray(inputs["c"], f32)
    pos = np.asarray(inputs["positions"], np.int32)
    rep = lambda v: np.ascontiguousarray(np.broadcast_to(np.asarray(v, f32).reshape(1, -1), (128, np.asarray(v).size)))
    col = lambda v: np.ascontiguousarray(np.asarray(v, f32).reshape(-1, 128).T)
    shared = {
        "w_ada": np.ascontiguousarray(inputs["w_ada"][0], f32), "b_ada_b": rep(inputs["b_ada"][0]),
        "norm_mix_col": col(inputs["norm_mix"][0]), "norm_ffn_b": rep(inputs["norm_ffn"][0]),
        "final_norm_b": rep(inputs["final_norm"]), "w_in": np.ascontiguousarray(inputs["w_in"][0], f32),
        "sinks_b": rep(inputs["sinks"][0]), "q_norm_col": col(inputs["q_norm"][0]), "kv_norm_col": col(inputs["kv_norm"][0]),
        "w_uq": np.ascontiguousarray(inputs["w_uq"][0], f32), "w_uk": np.ascontiguousarray(inputs["w_uk"][0], f32),
        "w_uv": np.ascontiguousarray(inputs["w_uv"][0], f32), "w_ba": np.ascontiguousarray(inputs["w_branch_a"][0], f32),
        "w_bb": np.ascontiguousarray(inputs["w_branch_b"][0], f32), "w_out": np.ascontiguousarray(inputs["w_out"][0], f32),
        "w_router": np.ascontiguousarray(inputs["w_router"][0], f32), "b_router_b": rep(inputs["b_router"][0]),
        "w_moe1": np.ascontiguousarray(inputs["w_moe1"][0], f32),
        "b1c": np.ascontiguousarray(np.asarray(inputs["b_moe1"][0], f32).reshape(32, 16, 128).transpose(0, 2, 1)),
        "w_moe2": np.ascontiguousarray(inputs["w_moe2"][0], f32), "b_moe2": np.ascontiguousarray(inputs["b_moe2"][0], f32),
    }
    shared["ident"] = np.eye(128, dtype=f32)
    t = np.arange(128)
    shared["ut"] = (t[:, None] < t[None, :]).astype(f32)
    shared["iota_j"] = np.ascontiguousarray(np.broadcast_to(np.arange(256, dtype=f32)[None, :], (128, 256)))
    slopes = np.exp2(-8.0 * np.arange(1, 9) / 8).astype(f32)
    k = t[:, None].astype(f32)
    q = t[None, :].astype(f32)
    tab = np.zeros((128, 2, 8, 128), f32)
    for typ in range(2):
        dist = q - k + (128.0 if typ == 0 else 0.0)
        valid = (dist >= 0) & (dist < 128)
        for h in range(8):
            tab[:, typ, h, :] = np.where(valid, -slopes[h] * dist, NEG)
    shared["swa_tab"] = tab.reshape(128, -1)
    freqs = (10000.0 ** (-np.arange(0, 32, 2, dtype=f32) / f32(32))).astype(f32)
    shared["freq_b"] = np.ascontiguousarray(np.broadcast_to(np.tile(freqs, 4)[None, :], (128, 64)))
    shared["kidx"] = np.ascontiguousarray((np.arange(64)[None, :] * 128 + t[:, None]).astype(f32))
    in_maps = []
    own = []
    for core in range(NCORES):
        b, j = core // 4, core % 4
        sbs = [j, 7 - j, 8 + j, 15 - j]
        xq = np.zeros((4, 640, D), f32)
        tok = []
        halo = np.zeros((128, 16), f32)
        for i, s in enumerate(sbs):
            st_ = s * 512
            if st_ >= 128:
                xq[i, 0:128] = x[b, st_ - 128:st_]
            else:
                halo[:, 4 * i] = NEG
            xq[i, 128:640] = x[b, st_:st_ + 512]
            tok.append(np.arange(st_, st_ + 512))
        tok = np.concatenate(tok)
        own.append((b, tok))
        m = dict(shared)
        m["xq"] = xq
        m["xall"] = np.ascontiguousarray(x[b])
        m["posq"] = np.ascontiguousarray(pos[b, tok].reshape(16, 128).T)
        m["posall"] = np.ascontiguousarray(pos[b].reshape(64, 128).T)
        m["qidx"] = np.ascontiguousarray(np.broadcast_to(tok.astype(f32)[None, :], (128, 2048)))
        m["halo_bias"] = halo
        m["c_col"] = col(c[b])
        in_maps.append(m)
    return in_maps, own


_NC_CACHE = {}


def kernel(**inputs):
    in_maps, own = _host_inputs(inputs)
    if "nc" not in _NC_CACHE:
        _NC_CACHE["nc"] = build_program()
    res = run_bass_kernel_spmd(_NC_CACHE["nc"], in_maps, core_ids=list(range(NCORES)))
    out = np.zeros((2, S, D), np.float32)
    for core in range(NCORES):
        b, tok = own[core]
        out[b, tok] = res.results[core]["out"]
    return out
```

```python
import numpy as np
import concourse.bass as bass
import concourse.mybir as mybir
from concourse.bass_utils import run_bass_kernel_spmd
from contextlib import ExitStack

F32 = mybir.dt.float32
BF16 = mybir.dt.bfloat16
I32 = mybir.dt.int32
AF = mybir.ActivationFunctionType
ALU = mybir.AluOpType

ENGS = ('pe', 'act', 'dve', 'pool', 'sp')
SEM_LIMIT = 12000
NEG = -30000.0
D = 1024
S = 8192
NCORES = 8
EPS = 1e-6
TWO_PI = 6.283185307179586
PI = 3.141592653589793


class Buf:
    __slots__ = ('w', 'r')

    def __init__(self):
        self.w = None
        self.r = []


def bufs(n):
    return [Buf() for _ in range(n)]


class Prog:
    def __init__(self, nc, stack, n_dma_sems=48):
        self.nc = nc
        self.stack = stack
        self.streams = {e: [] for e in ENGS}
        self.cur = {}
        self.cnt = {}
        self.last = {e: None for e in ENGS}
        self.known = {e: {} for e in ENGS}
        self.nsem = 0
        for e in ENGS:
            self._new_eng_sem(e)
        self.dma_sems = []
        for i in range(n_dma_sems):
            s = stack.enter_context(nc.semaphore(f"dq{i}"))
            self.dma_sems.append([s, 0, None])
        self.dma_rr = 0

    def _new_eng_sem(self, e):
        s = self.stack.enter_context(self.nc.semaphore(f"s_{e}_{self.nsem}"))
        self.nsem += 1
        self.cur[e] = s
        self.cnt[e] = 0

    def _waits_for(self, e, deps):
        need = {}
        for tok in deps:
            if tok is None:
                continue
            sem, val = tok
            k = id(sem)
            if k not in need or need[k][1] < val:
                need[k] = (sem, val)
        waits = []
        kn = self.known[e]
        for k, (sem, val) in need.items():
            if kn.get(k, 0) < val:
                waits.append((sem, val))
                kn[k] = val
        return waits

    @staticmethod
    def _deps(reads, writes):
        deps = []
        for b in reads:
            deps.append(b.w)
        for b in writes:
            deps.append(b.w)
            deps.extend(b.r)
        return deps

    @staticmethod
    def _mark(tok, reads, writes):
        for b in reads:
            b.r = [t for t in b.r if t[0] is not tok[0]] + [tok]
        for b in writes:
            b.w = tok
            b.r = []

    def op(self, e, fn, reads=(), writes=()):
        deps = self._deps(reads, writes)
        if e == 'pe':
            deps = [t for t in deps if t is not None and t[0] is not self.cur['pe']]
        waits = self._waits_for(e, deps)
        if self.cnt[e] >= SEM_LIMIT:
            self._new_eng_sem(e)
        self.cnt[e] += 1
        tok = (self.cur[e], self.cnt[e])
        self.last[e] = tok
        self.streams[e].append((waits, fn, tok, 1))
        self._mark(tok, reads, writes)
        return tok

    def dma(self, q, out, in_, reads=(), writes=()):
        deps = self._deps(reads, writes)
        ent = self.dma_sems[self.dma_rr]
        self.dma_rr = (self.dma_rr + 1) % len(self.dma_sems)
        if ent[2] is not None:
            deps.append(ent[2])
        waits = self._waits_for(q, deps)
        ent[1] += 16
        tok = (ent[0], ent[1])
        ent[2] = tok
        fn = lambda eng, o=out, i=in_: eng.dma_start(out=o, in_=i)
        self.streams[q].append((waits, fn, tok, 16))
        self._mark(tok, reads, writes)
        return tok

    def wait_all(self, e, toks):
        waits = self._waits_for(e, toks)
        if waits:
            self.streams[e].append((waits, None, None, 0))

    def barrier(self):
        toks = [self.last[e] for e in ENGS] + [ent[2] for ent in self.dma_sems]
        for e in ENGS:
            self.wait_all(e, toks)

    def finish(self):
        nc = self.nc
        with nc.Block() as block:
            def run(eng, items):
                for waits, fn, tok, inc in items:
                    for sem, val in waits:
                        eng.wait_ge(sem, val)
                    if fn is not None:
                        fn(eng).then_inc(tok[0], inc)

            @block.tensor
            def _(eng):
                run(eng, self.streams['pe'])

            @block.scalar
            def _(eng):
                run(eng, self.streams['act'])

            @block.vector
            def _(eng):
                run(eng, self.streams['dve'])

            @block.gpsimd
            def _(eng):
                run(eng, self.streams['pool'])

            @block.sync
            def _(eng):
                run(eng, self.streams['sp'])


class Ops:
    def __init__(self, P):
        self.P = P

    def mm(self, out, lhsT, rhs, start, stop, R, W):
        return self.P.op('pe', lambda e: e.matmul(out, lhsT, rhs, start=start, stop=stop), R, W)

    def tr(self, out, in_, ident, R, W):
        return self.P.op('pe', lambda e: e.transpose(out, in_, ident), R, W)

    def act(self, out, in_, func, R, W, **kw):
        return self.P.op('act', lambda e: e.activation(out=out, in_=in_, func=func, **kw), R, W)

    def ts(self, eng, out, in0, s1, s2, op0, op1, R, W, **kw):
        if op1 is None:
            return self.P.op(eng, lambda e: e.tensor_scalar(out, in0, s1, None, op0, **kw), R, W)
        return self.P.op(eng, lambda e: e.tensor_scalar(out, in0, s1, s2, op0, op1, **kw), R, W)

    def tt(self, eng, out, in0, in1, op, R, W):
        return self.P.op(eng, lambda e: e.tensor_tensor(out, in0, in1, op), R, W)

    def stt(self, out, in0, scalar, in1, op0, op1, R, W, **kw):
        return self.P.op('dve', lambda e: e.scalar_tensor_tensor(out, in0, scalar, in1, op0, op1, **kw), R, W)

    def cp(self, eng, out, in_, R, W):
        if eng == 'act':
            return self.P.op('act', lambda e: e.copy(out, in_), R, W)
        return self.P.op(eng, lambda e: e.tensor_copy(out, in_), R, W)

    def memset(self, eng, ap, val, W):
        return self.P.op(eng, lambda e: e.memset(ap, val), (), W)

    def recip(self, out, in_, R, W):
        return self.P.op('dve', lambda e: e.reciprocal(out, in_), R, W)

    def max8(self, out, in_, R, W):
        return self.P.op('dve', lambda e: e.max(out, in_), R, W)

    def dma(self, q, out, in_, R, W):
        return self.P.dma(q, out, in_, R, W)


IN_SPECS = [
    ("xq", [4, 640, D], F32), ("xall", [S, D], F32),
    ("posq", [128, 16], I32), ("posall", [128, 64], I32),
    ("qidx", [128, 2048], F32), ("kidx", [128, 64], F32), ("halo_bias", [128, 16], F32),
    ("c_col", [128, 8], F32), ("w_ada", [D, 6 * D], F32), ("b_ada_b", [128, 6 * D], F32),
    ("norm_mix_col", [128, 8], F32), ("norm_ffn_b", [128, D], F32), ("final_norm_b", [128, D], F32),
    ("w_in", [D, 3488], F32), ("sinks_b", [128, 8], F32),
    ("q_norm_col", [128, 3], F32), ("kv_norm_col", [128, 2], F32),
    ("w_uq", [384, 768], F32), ("w_uk", [256, 512], F32), ("w_uv", [256, 512], F32),
    ("w_ba", [512, D], F32), ("w_bb", [512, D], F32), ("w_out", [D, D], F32),
    ("w_router", [D, 32], F32), ("b_router_b", [128, 32], F32),
    ("w_moe1", [32, D, 2048], F32), ("b1c", [32, 128, 16], F32),
    ("w_moe2", [32, D, D], F32), ("b_moe2", [32, D], F32),
    ("ident", [128, 128], F32), ("ut", [128, 128], F32), ("iota_j", [128, 256], F32),
    ("swa_tab", [128, 2 * 8 * 128], F32), ("freq_b", [128, 64], F32),
]


def build_program(n_experts=32, phases=6):
    nc = bass.Bass("TRN2", target_bir_lowering=False)
    A = {}
    for name, shape, dt in IN_SPECS:
        A[name] = nc.dram_tensor(name, shape, dt, kind="ExternalInput").ap()
    out_d = nc.dram_tensor("out", [2048, D], F32, kind="ExternalOutput").ap()
    za_d = nc.dram_tensor("za_d", [16, 128, D], BF16, kind="Internal").ap()
    qT_d = nc.dram_tensor("qT_d", [8, 96, 2048], BF16, kind="Internal").ap()
    ckvT_d = nc.dram_tensor("ckvT_d", [2, 128, S], BF16, kind="Internal").ap()
    krT_d = nc.dram_tensor("krT_d", [32, S], BF16, kind="Internal").ap()
    obT_d = nc.dram_tensor("obT_d", [8, 64, 2048], BF16, kind="Internal").ap()
    Bza_d, BqT_d, Bckv_d, Bkr_d, Bob_d = bufs(5)
    SCALE_MLA = 96.0 ** -0.5

    with ExitStack() as gs:
        P = Prog(nc, gs)
        O = Ops(P)

        _cnt = [0]

        def sb(st, name, shape, dt):
            _cnt[0] += 1
            return st.enter_context(nc.sbuf_tensor(f"sb{_cnt[0]}_{name}", shape, dt))

        ps = [gs.enter_context(nc.psum_tensor(f"ps{i}", [128, 512], F32)) for i in range(8)]
        psb = bufs(8)

        ident_f = sb(gs, "ident_f", [128, 128], F32)
        ident_b = sb(gs, "ident_b", [128, 128], BF16)
        ones_b = sb(gs, "ones_b", [128, 128], BF16)
        Bc = Buf()
        O.dma('sp', ident_f[:], A["ident"], (), [Bc])
        O.cp('dve', ident_b[:], ident_f[:], [Bc], [Bc])
        O.memset('dve', ones_b[:], 1.0, [Bc])

        g1B = sb(gs, "g1B", [128, D], F32)
        G2B = sb(gs, "G2B", [128, D], F32)
        sh2B = sb(gs, "sh2B", [128, D], F32)
        g2B = sb(gs, "g2B", [128, D], F32)
        G1col = sb(gs, "G1col", [128, 8], F32)
        sh1col = sb(gs, "sh1col", [128, 8], BF16)
        Bmod = Buf()
        xt = [sb(gs, f"xt{i}", [128, D], F32) for i in range(2)]
        xtb = bufs(2)
        xh = sb(gs, "xh", [128, D], BF16)
        xhT = sb(gs, "xhT", [128, 8, 128], BF16)
        junk = sb(gs, "junk", [128, D], BF16)
        sml = sb(gs, "sml", [128, 8], F32)
        Bx = Buf()
        xh_1 = sb(gs, "xh_1", [128, D], BF16)
        xhT_1 = sb(gs, "xhT_1", [128, 8, 128], BF16)
        junk_1 = junk
        sml_1 = sb(gs, "sml_1", [128, 8], F32)
        Bx_1 = Buf()
        FE = [(xh, xhT, junk, sml, Bx), (xh_1, xhT_1, junk_1, sml_1, Bx_1)]
        E = {"ot": [sb(gs, f"ot{i}", [128, 512], F32) for i in range(2)],
             "den": [sb(gs, f"den{i}", [64, 512], F32) for i in range(2)],
             "b": bufs(2)}

        def rstd_of(src_ap, n, jk, ssv, rs, R, Bt, psum=False):
            if psum:
                O.act(jk, src_ap, AF.Square, R, [Bt], accum_out=ssv)
            else:
                O.stt(jk, src_ap, 1.0, src_ap, ALU.mult, ALU.mult, R, [Bt], accum_out=ssv)
            O.ts('dve', ssv, ssv, 1.0 / n, EPS, ALU.mult, ALU.add, [Bt], [Bt])
            O.act(ssv, ssv, AF.Ln, [Bt], [Bt])
            O.act(rs, ssv, AF.Exp, [Bt], [Bt], scale=-0.5)

        def fe_a(src_dram_ap, slot, fs=0):
            xh_, xhT_, junk_, sml_, Bx_ = FE[fs]
            O.dma('sp', xt[slot][:], src_dram_ap, (), [xtb[slot]])
            rstd_of(xt[slot][:], D, junk_[:], sml_[:, 0:1], sml_[:, 1:2], [xtb[slot]], Bx_)

        def fe_b1(slot, fs=0):
            xh_, xhT_, junk_, sml_, Bx_ = FE[fs]
            O.act(xh_[:], xt[slot][:], AF.Copy, [xtb[slot], Bx_], [Bx_], scale=sml_[:, 1:2])

        def fe_b2(fs=0, pbank=0):
            xh_, xhT_, junk_, sml_, Bx_ = FE[fs]
            pT = ps[pbank][:].bitcast(BF16)
            for k in range(8):
                O.tr(pT[:, k * 128:(k + 1) * 128], xh_[:, k * 128:(k + 1) * 128], ident_b[:], [Bx_, Bc], [psb[pbank]])
            O.cp('dve' if fs else 'act', xhT_[:].rearrange("p k t -> p (k t)"), pT, [psb[pbank]], [Bx_])

        def front_end(src_dram_ap, slot, fs=0, pbank=0):
            xh_, xhT_, junk_, sml_, Bx_ = FE[fs]
            O.dma('sp', xt[slot][:], src_dram_ap, (), [xtb[slot]])
            rstd_of(xt[slot][:], D, junk_[:], sml_[:, 0:1], sml_[:, 1:2], [xtb[slot]], Bx_)
            O.act(xh_[:], xt[slot][:], AF.Copy, [xtb[slot], Bx_], [Bx_], scale=sml_[:, 1:2])
            pT = ps[pbank][:].bitcast(BF16)
            for k in range(8):
                O.tr(pT[:, k * 128:(k + 1) * 128], xh_[:, k * 128:(k + 1) * 128], ident_b[:], [Bx_, Bc], [psb[pbank]])
            O.cp('dve' if fs else 'act', xhT_[:].rearrange("p k t -> p (k t)"), pT, [psb[pbank]], [Bx_])

        sW1 = gs.enter_context(ExitStack())
        W1 = sb(sW1, "W1", [128, 8, 2176], BF16)
        bW1 = Buf()
        wuq = sb(sW1, "wuq", [128, 3, 768], BF16)
        wba = sb(sW1, "wba", [64, 8, D], BF16)
        Bw = Buf()
        w_in_v0 = A["w_in"].rearrange("(c p) n -> p c n", p=128)
        O.dma('pool', W1[:, :, 0:1152], w_in_v0[:, :, 0:1152], (), [bW1])
        O.dma('pool', W1[:, :, 1152:2176], w_in_v0[:, :, 1440:2464], (), [bW1])
        O.dma('pool', wuq[:], A["w_uq"].rearrange("(c p) n -> p c n", p=128), (), [Bw])
        O.dma('pool', wba[:], A["w_ba"].rearrange("(h p) n -> p h n", p=64), (), [Bw])
        with ExitStack() as st:
            ones_f = sb(st, "ones_f", [128, 128], F32)
            O.memset('dve', ones_f[:], 1.0, [Bc])
            ccol = sb(st, "ccol", [128, 8], F32)
            cact = sb(st, "cact", [128, 8], F32)
            crep = sb(st, "crep", [128, 8, 128], BF16)
            modB = sb(st, "modB", [128, 6 * D], F32)
            badaB = sb(st, "badaB", [128, 6 * D], F32)
            nmcol = sb(st, "nmcol", [128, 8], F32)
            nfB = sb(st, "nfB", [128, D], F32)
            wada = [sb(st, f"wada{i}", [128, 8, 512], BF16) for i in range(3)]
            wadab = bufs(3)
            sc1col = sb(st, "sc1col", [128, 8], F32)
            sh1f = sb(st, "sh1f", [128, 8], F32)
            B0 = Buf()
            O.dma('sp', ccol[:], A["c_col"], (), [B0])
            O.dma('sp', badaB[:], A["b_ada_b"], (), [B0])
            O.dma('sp', nmcol[:], A["norm_mix_col"], (), [B0])
            O.dma('sp', nfB[:], A["norm_ffn_b"], (), [B0])
            O.act(cact[:], ccol[:], AF.Silu, [B0], [B0])
            for k in range(8):
                O.act(crep[:, k, :], ones_f[:], AF.Copy, [B0, Bc], [B0], scale=cact[:, k:k + 1])
            wv = A["w_ada"].rearrange("(c p) n -> p c n", p=128)
            for n in range(12):
                sl = n % 3
                O.dma('pool', wada[sl][:], wv[:, :, n * 512:(n + 1) * 512], (), [wadab[sl]])
                pb = n % 2
                for k in range(8):
                    O.mm(ps[pb][:], crep[:, k, :], wada[sl][:, k, :], k == 0, k == 7, [B0, wadab[sl]], [psb[pb]])
                O.tt('dve', modB[:, n * 512:(n + 1) * 512], ps[pb][:], badaB[:, n * 512:(n + 1) * 512], ALU.add,
                     [psb[pb], B0], [B0])
            for which, dst in ((0, sh1f), (1, sc1col)):
                for c in range(8):
                    pb = 2 + (c % 2)
                    O.tr(ps[pb][:, 0:128], modB[:, which * D + c * 128: which * D + (c + 1) * 128], ident_f[:],
                         [B0, Bc], [psb[pb]])
                    O.cp('dve', dst[:, c:c + 1], ps[pb][:, 0:1], [psb[pb]], [B0])
            O.cp('dve', sh1col[:], sh1f[:], [B0], [Bmod])
            O.ts('dve', sc1col[:], sc1col[:], 1.0, None, ALU.add, None, [B0], [B0])
            O.tt('dve', G1col[:], sc1col[:], nmcol[:], ALU.mult, [B0], [Bmod])
            O.cp('dve', g1B[:], modB[:, 2 * D:3 * D], [B0], [Bmod])
            O.cp('dve', sh2B[:], modB[:, 3 * D:4 * D], [B0], [Bmod])
            O.ts('dve', G2B[:], modB[:, 4 * D:5 * D], 1.0, None, ALU.add, None, [B0], [Bmod])
            O.tt('dve', G2B[:], G2B[:], nfB[:], ALU.mult, [Bmod, B0], [Bmod])
            O.cp('dve', g2B[:], modB[:, 5 * D:6 * D], [B0], [Bmod])
            P.barrier()

        w_in_v = A["w_in"].rearrange("(c p) n -> p c n", p=128)

        def load_win_dma(wt, wb, col_ranges):
            off = 0
            for (a, b) in col_ranges:
                O.dma('pool', wt[:, :, off:off + (b - a)], w_in_v[:, :, a:b], (), [wb])
                off += b - a
            return off

        def load_win(wt, wb, brow, col_ranges, ncols=None):
            if ncols is None:
                ncols = load_win_dma(wt, wb, col_ranges)
            for n0 in range(0, ncols, 512):
                n1 = min(ncols, n0 + 512)
                for k in range(8):
                    O.mm(ps[7][0:1, 0:n1 - n0], sh1col[:, k:k + 1], wt[:, k, n0:n1], k == 0, k == 7,
                         [Bmod, wb], [psb[7]])
                O.cp('dve', brow[0:1, n0:n1], ps[7][0:1, 0:n1 - n0], [psb[7]], [wb])
            for k in range(8):
                O.act(wt[:, k, 0:ncols], wt[:, k, 0:ncols], AF.Copy, [Bmod, wb], [wb], scale=G1col[:, k:k + 1])

        def alloc_rope(st, pfx):
            T = {}
            for nm in ("ang", "u", "kf", "r", "m", "sin", "cos", "freq"):
                T[nm] = sb(st, pfx + nm, [128, 64], F32)
            T["posf"] = sb(st, pfx + "posf", [128, 4], F32)
            T["ki"] = sb(st, pfx + "ki", [128, 64], I32)
            return T

        def rope_tables(T, pos_i32_ap, Bt):
            O.cp('dve', T["posf"][:], pos_i32_ap, [Bt], [Bt])
            for j in range(4):
                O.ts('dve', T["ang"][:, j * 16:(j + 1) * 16], T["freq"][:, j * 16:(j + 1) * 16],
                     T["posf"][:, j:j + 1], None, ALU.mult, None, [Bt], [Bt])
            O.ts('dve', T["u"][:], T["ang"][:], 1.0 / TWO_PI, None, ALU.mult, None, [Bt], [Bt])
            O.cp('dve', T["ki"][:], T["u"][:], [Bt], [Bt])
            O.cp('dve', T["kf"][:], T["ki"][:], [Bt], [Bt])
            C1 = 6.28125
            C2 = TWO_PI - 6.28125
            O.stt(T["r"][:], T["kf"][:], -C1, T["ang"][:], ALU.mult, ALU.add, [Bt], [Bt])
            O.stt(T["r"][:], T["kf"][:], -C2, T["r"][:], ALU.mult, ALU.add, [Bt], [Bt])

            def wrap(x):
                O.ts('dve', T["m"][:], x, PI, -TWO_PI, ALU.is_gt, ALU.mult, [Bt], [Bt])
                O.tt('dve', x, x, T["m"][:], ALU.add, [Bt], [Bt])
                O.ts('dve', T["m"][:], x, -PI, TWO_PI, ALU.is_lt, ALU.mult, [Bt], [Bt])
                O.tt('dve', x, x, T["m"][:], ALU.add, [Bt], [Bt])
            wrap(T["r"][:])
            O.act(T["sin"][:], T["r"][:], AF.Sin, [Bt], [Bt])
            O.ts('dve', T["r"][:], T["r"][:], PI / 2, None, ALU.add, None, [Bt], [Bt])
            wrap(T["r"][:])
            O.act(T["cos"][:], T["r"][:], AF.Sin, [Bt], [Bt])

        def apply_rope(src, dst, nh, cosj, sinj, tmp, Rr, Bt, Wd):
            cb = cosj.unsqueeze(1).broadcast_to([128, nh, 16])
            sbb = sinj.unsqueeze(1).broadcast_to([128, nh, 16])
            t1 = src[:, :, 0:16]
            t2 = src[:, :, 16:32]
            a, b = tmp
            O.tt('dve', a, t1, cb, ALU.mult, Rr, [Bt])
            O.tt('dve', b, t2, sbb, ALU.mult, Rr, [Bt])
            O.tt('dve', dst[:, :, 0:16], a, b, ALU.subtract, [Bt], Wd)
            O.tt('dve', a, t2, cb, ALU.mult, Rr, [Bt])
            O.tt('dve', b, t1, sbb, ALU.mult, Rr, [Bt])
            O.tt('dve', dst[:, :, 16:32], a, b, ALU.add, [Bt], Wd)

        def attn_epilogue(pbank, pbuf, slot, dst_ap, extra_den, Wd):
            ot, den = E["ot"][slot], E["den"][slot]
            Bo = E["b"][slot]
            O.cp('act', ot[:], pbank[:], [pbuf], [Bo])
            O.dma('sp', den[0:64, :], ot[64:128, :], [Bo], [Bo])
            if extra_den is not None:
                O.tt('dve', den[0:64, :], den[0:64, :], extra_den, ALU.add, [Bo, Bc], [Bo])
            O.recip(den[0:64, :], den[0:64, :], [Bo], [Bo])
            if len(dst_ap.shape) == 3:
                hh = dst_ap.shape[1]
                O.tt('dve', dst_ap, ot[0:64, :].rearrange("p (h q) -> p h q", h=hh),
                     den[0:64, :].rearrange("p (h q) -> p h q", h=hh), ALU.mult, [Bo], Wd)
            else:
                O.tt('dve', dst_ap, ot[0:64, :], den[0:64, :], ALU.mult, [Bo], Wd)

        if phases >= 1:
          with ExitStack() as st:
            brow1 = sb(st, "brow1", [1, 2176], BF16)
            load_win(W1, bW1, brow1, None, ncols=2176)
            qncol = sb(st, "qncol", [128, 3], F32)
            O.dma('sp', qncol[:], A["q_norm_col"], (), [Bw])
            for c in range(3):
                O.act(wuq[:, c, :], wuq[:, c, :], AF.Copy, [Bw], [Bw], scale=qncol[:, c:c + 1])
            zeros_f = sb(st, "zeros_f", [128, 128], F32)
            O.memset('dve', zeros_f[:], 0.0, [Bc])
            swat = sb(st, "swat", [128, 2, 8, 128], F32)
            halo = sb(st, "halo", [128, 16], F32)
            sinksB = sb(st, "sinksB", [128, 8], F32)
            esink = sb(st, "esink", [64, 8, 128], F32)
            posq = sb(st, "posq", [128, 16], I32)
            RT = alloc_rope(st, "rq_")
            Bt = Buf()
            O.dma('sp', swat[:].rearrange("p a h q -> p (a h q)"), A["swa_tab"], (), [Bc])
            O.dma('sp', halo[:], A["halo_bias"], (), [Bc])
            O.dma('sp', sinksB[:], A["sinks_b"], (), [Bc])
            O.dma('sp', posq[:], A["posq"], (), [Bt])
            O.dma('sp', RT["freq"][:], A["freq_b"], (), [Bt])
            for h in range(8):
                O.act(esink[:, h, :], zeros_f[0:64, :], AF.Exp, [Bc], [Bc], bias=sinksB[0:64, h:h + 1])
            KTs = sb(st, "KTs", [128, 5, 128], BF16)
            VOs = sb(st, "VOs", [128, 5, 2, 128], BF16)
            QTs = sb(st, "QTs", [128, 4, 4, 128], BF16)
            siga = sb(st, "siga", [128, 4, D], BF16)
            OAT = sb(st, "OAT", [64, 8, 512], BF16)
            QTm = sb(st, "QTm", [96, 8, 512], BF16)
            zat = sb(st, "zat", [128, D], BF16)
            Bsb = Buf()
            Bzat = Buf()
            BQTm = Buf()
            O.memset('pool', VOs[:, :, :, 64:128], 1.0, [Bsb])
            qa_tm = sb(st, "qa_tm", [128, 512], BF16)
            kv_tm = sb(st, "kv_tm", [128, 128], BF16)
            cqn = sb(st, "cqn", [128, 384], BF16)
            cqnT = sb(st, "cqnT", [128, 3, 128], BF16)
            q_tm = sb(st, "q_tm", [128, 8, 96], BF16)
            qst = sb(st, "qst", [128, 768], F32)
            rtmp = [sb(st, f"rtmp{i}", [128, 8, 16], F32) for i in range(2)]
            stmp = sb(st, "stmp", [128, 512], F32)
            PT = [sb(st, f"PT{i}", [128, 512], BF16) for i in range(2)]
            PTb = bufs(2)
            Bq = Buf()
            pT6 = ps[6][:].bitcast(BF16)
            pT7 = ps[7][:].bitcast(BF16)
            for i in range(4):
                rope_tables(RT, posq[:, 4 * i:4 * i + 4], Bt)
                for r in range(5):
                    front_end(A["xq"][i, r * 128:(r + 1) * 128, :], r % 2)
                    if r == 0:
                        for k in range(8):
                            O.mm(ps[1][:, 0:256], xhT[:, k, :], W1[:, k, 512:768], k == 0, False, [Bx, bW1], [psb[1]])
                        O.mm(ps[1][:, 0:256], ones_b[0:1, :], brow1[0:1, 512:768], False, True, [Bc, bW1], [psb[1]])
                        kps, kbuf = ps[1], psb[1]
                    else:
                        for (pb, c0, c1) in ((1, 0, 512), (2, 512, 1024), (3, 1024, 1152)):
                            for k in range(8):
                                O.mm(ps[pb][:, 0:c1 - c0], xhT[:, k, :], W1[:, k, c0:c1], k == 0, False, [Bx, bW1], [psb[pb]])
                            O.mm(ps[pb][:, 0:c1 - c0], ones_b[0:1, :], brow1[0:1, c0:c1], False, True, [Bc, bW1], [psb[pb]])
                        for hf in range(2):
                            pb = 4 + hf
                            c0 = 1152 + hf * 512
                            for k in range(8):
                                O.mm(ps[pb][:], xhT[:, k, :], W1[:, k, c0:c0 + 512], k == 0, False, [Bx, bW1], [psb[pb]])
                            O.mm(ps[pb][:], ones_b[0:1, :], brow1[0:1, c0:c0 + 512], False, True, [Bc, bW1], [psb[pb]])
                            O.act(siga[:, r - 1, hf * 512:(hf + 1) * 512], ps[pb][:], AF.Sigmoid, [psb[pb]], [Bsb])
                        kps, kbuf = ps[2], psb[2]
                    O.cp('dve', kv_tm[:], kps[:, 0:128], [kbuf], [Bq])
                    O.cp('dve', VOs[:, r, :, 0:64], kps[:, 128:256].rearrange("p (g d) -> p g d", g=2), [kbuf], [Bsb])
                    O.tr(pT6[:, 0:128], kv_tm[:], ident_b[:], [Bq, Bc], [psb[6]])
                    O.cp('dve', KTs[:, r, :], pT6[:, 0:128], [psb[6]], [Bsb])
                    if r == 0:
                        continue
                    O.cp('act', qa_tm[:].rearrange("p (a g d) -> p g a d", a=4, g=2),
                         ps[1][:].rearrange("p (g a d) -> p g a d", g=2, a=4), [psb[1]], [Bq])
                    for a in range(4):
                        O.tr(pT6[:, 128 + a * 128:128 + (a + 1) * 128], qa_tm[:, a * 128:(a + 1) * 128], ident_b[:], [Bq, Bc], [psb[6]])
                    O.cp('act', QTs[:, r - 1, :, :].rearrange("p a q -> p (a q)"), pT6[:, 128:640], [psb[6]], [Bsb])
                    O.act(junk[:, 0:256], ps[2][:, 256:512], AF.Square, [psb[2]], [Bq], accum_out=sml[:, 2:3])
                    O.act(junk[:, 256:384], ps[3][:, 0:128], AF.Square, [psb[3]], [Bq], accum_out=sml[:, 3:4])
                    O.tt('dve', sml[:, 2:3], sml[:, 2:3], sml[:, 3:4], ALU.add, [Bq], [Bq])
                    O.ts('dve', sml[:, 2:3], sml[:, 2:3], 1.0 / 384, EPS, ALU.mult, ALU.add, [Bq], [Bq])
                    O.act(sml[:, 2:3], sml[:, 2:3], AF.Ln, [Bq], [Bq])
                    O.act(sml[:, 4:5], sml[:, 2:3], AF.Exp, [Bq], [Bq], scale=-0.5)
                    O.act(cqn[:, 0:256], ps[2][:, 256:512], AF.Copy, [psb[2], Bq], [Bq], scale=sml[:, 4:5])
                    O.act(cqn[:, 256:384], ps[3][:, 0:128], AF.Copy, [psb[3], Bq], [Bq], scale=sml[:, 4:5])
                    for c in range(3):
                        O.tr(pT7[:, c * 128:(c + 1) * 128], cqn[:, c * 128:(c + 1) * 128], ident_b[:], [Bq, Bc], [psb[7]])
                    O.cp('act', cqnT[:].rearrange("p c t -> p (c t)"), pT7[:, 0:384], [psb[7]], [Bq])
                    for (pb, c0, c1) in ((2, 0, 512), (3, 512, 768)):
                        for c in range(3):
                            O.mm(ps[pb][:, 0:c1 - c0], cqnT[:, c, :], wuq[:, c, c0:c1], c == 0, c == 2, [Bq, Bw], [psb[pb]])
                    O.cp('act', qst[:, 0:512], ps[2][:], [psb[2]], [Bq])
                    O.cp('act', qst[:, 512:768], ps[3][:, 0:256], [psb[3]], [Bq])
                    qs3 = qst[:].rearrange("p (h d) -> p h d", h=8)
                    O.cp('pool', q_tm[:, :, 0:64], qs3[:, :, 0:64], [Bq], [Bq])
                    j = r - 1
                    apply_rope(qs3[:, :, 64:96], q_tm[:, :, 64:96], 8, RT["cos"][:, j * 16:(j + 1) * 16],
                               RT["sin"][:, j * 16:(j + 1) * 16], (rtmp[0][:], rtmp[1][:]), [Bq, Bt], Bq, [Bq])
                    for h in range(8):
                        O.tr(pT7[0:96, h * 128:(h + 1) * 128], q_tm[:, h, :], ident_b[:], [Bq, Bc], [psb[7]])
                    O.cp('act', QTm[:, :, j * 128:(j + 1) * 128], pT7[0:96, :].rearrange("p (h t) -> p h t", h=8),
                         [psb[7]], [BQTm])
                O.dma('sp', qT_d[:, :, i * 512:(i + 1) * 512].rearrange("h p t -> p h t"), QTm[:], [BQTm], [BqT_d])
                for r in range(1, 5):
                    ti = 4 * i + (r - 1)
                    for g in range(2):
                        pacc = 4 + g
                        for typ, kt in ((0, r - 1), (1, r)):
                            pb = 1 + typ
                            O.mm(ps[pb][:], KTs[64 * g:64 * g + 64, kt, :],
                                 QTs[64 * g:64 * g + 64, r - 1, :, :].rearrange("p a q -> p (a q)"),
                                 True, True, [Bsb], [psb[pb]])
                            O.stt(stmp[:], ps[pb][:], 0.125,
                                  swat[:, typ, 4 * g:4 * g + 4, :].rearrange("p h q -> p (h q)"),
                                  ALU.mult, ALU.add, [psb[pb], Bc], [Bq])
                            sl = typ
                            if typ == 0:
                                O.act(PT[sl][:], stmp[:], AF.Exp, [Bq, Bc], [PTb[sl]], bias=halo[:, ti:ti + 1])
                            else:
                                O.act(PT[sl][:], stmp[:], AF.Exp, [Bq], [PTb[sl]])
                            O.mm(ps[pacc][:], VOs[:, kt, g, :], PT[sl][:], typ == 0, typ == 1, [Bsb, PTb[sl]], [psb[pacc]])
                        attn_epilogue(ps[pacc], psb[pacc], g,
                                      OAT[:, 4 * g:4 * g + 4, (r - 1) * 128:r * 128],
                                      esink[:, 4 * g:4 * g + 4, :].rearrange("p h q -> p (h q)"), [Bsb])
                    for hf in range(2):
                        pb = 6 + hf
                        for h in range(8):
                            O.mm(ps[pb][:], OAT[:, h, (r - 1) * 128:r * 128], wba[:, h, hf * 512:(hf + 1) * 512],
                                 h == 0, h == 7, [Bsb, Bw], [psb[pb]])
                        O.tt('dve', zat[:, hf * 512:(hf + 1) * 512], ps[pb][:], siga[:, r - 1, hf * 512:(hf + 1) * 512],
                             ALU.mult, [psb[pb], Bsb], [Bzat])
                    O.dma('sp', za_d[ti], zat[:], [Bzat], [Bza_d])
            P.barrier()

        sW1.close()

        if phases >= 2:
          with ExitStack() as st:
            W2 = sb(st, "W2", [128, 8, 288], BF16)
            bW2 = Buf()
            brow2 = sb(st, "brow2", [1, 288], BF16)
            load_win(W2, bW2, brow2, [(1152, 1440)])
            posall = sb(st, "posall", [128, 64], I32)
            RT = alloc_rope(st, "rk_")
            Bt = Buf()
            O.dma('sp', posall[:], A["posall"], (), [Bt])
            O.dma('sp', RT["freq"][:], A["freq_b"], (), [Bt])
            ckvn = sb(st, "ckvn", [128, 256], BF16)
            kr_tm = sb(st, "kr_tm", [128, 1, 32], BF16)
            ktmp = [sb(st, f"ktmp{i}", [128, 1, 16], F32) for i in range(2)]
            ckvT_st = sb(st, "ckvT_st", [128, 2, 512], BF16)
            krT_st = sb(st, "krT_st", [32, 512], BF16)
            Bk = Buf()
            Bst = Buf()
            pT6 = ps[6][:].bitcast(BF16)
            pT7 = ps[7][:].bitcast(BF16)
            ckvn2 = [ckvn, sb(st, "ckvn_1", [128, 256], BF16)]
            kr_tm2 = [kr_tm, sb(st, "kr_tm_1", [128, 1, 32], BF16)]
            Bk2 = [Bk, Buf()]
            sml2 = [sml, sml_1]

            def xsrc(t):
                return A["xall"][t * 128:(t + 1) * 128, :]

            def d2(t):
                grp, j = t // 4, t % 4
                f = t % 2
                for c in range(2):
                    O.tr(pT6[:, (2 * j + c) * 128:(2 * j + c + 1) * 128], ckvn2[f][:, c * 128:(c + 1) * 128], ident_b[:],
                         [Bk2[f], Bc], [psb[6]])
                O.tr(pT7[0:32, j * 128:(j + 1) * 128], kr_tm2[f][:, 0, :], ident_b[:], [Bk2[f], Bc], [psb[7]])
                if j == 3:
                    O.cp('act', ckvT_st[:].rearrange("p c (j t) -> p j c t", j=4),
                         pT6[:, 0:1024].rearrange("p (j c t) -> p j c t", j=4, c=2), [psb[6]], [Bst])
                    O.cp('dve', krT_st[:], pT7[0:32, 0:512], [psb[7]], [Bst])
                    O.dma('sp', ckvT_d[:, :, grp * 512:(grp + 1) * 512].rearrange("c p t -> p c t"), ckvT_st[:], [Bst], [Bckv_d])
                    O.dma('sp', krT_d[:, grp * 512:(grp + 1) * 512], krT_st[:], [Bst], [Bkr_d])
            fe_a(xsrc(0), 0, 0)
            fe_a(xsrc(1), 1, 1)
            fe_b1(0, 0)
            fe_b2(0, 0)
            for t in range(64):
                grp, j = t // 4, t % 4
                f = t % 2
                xhT_c, Bx_c = FE[f][1], FE[f][4]
                if j == 0:
                    rope_tables(RT, posall[:, 4 * grp:4 * grp + 4], Bt)
                if t + 1 < 64:
                    fe_b1((t + 1) % 2, (t + 1) % 2)
                pp = 1 if f == 0 else 3
                for k in range(8):
                    O.mm(ps[pp][:, 0:288], xhT_c[:, k, :], W2[:, k, :], k == 0, False, [Bx_c, bW2], [psb[pp]])
                O.mm(ps[pp][:, 0:288], ones_b[0:1, :], brow2[0:1, :], False, True, [Bc, bW2], [psb[pp]])
                if t + 1 < 64:
                    fe_b2((t + 1) % 2, 0 if (t + 1) % 2 == 0 else 2)
                if t + 2 < 64:
                    fe_a(xsrc(t + 2), (t + 2) % 2, (t + 2) % 2)
                rstd_of(ps[pp][:, 0:256], 256, junk[:, 0:256], sml2[f][:, 2:3], sml2[f][:, 4:5], [psb[pp]], Bk2[f], psum=True)
                O.act(ckvn2[f][:], ps[pp][:, 0:256], AF.Copy, [psb[pp], Bk2[f]], [Bk2[f]], scale=sml2[f][:, 4:5])
                apply_rope(ps[pp][:, 256:288].rearrange("p (h d) -> p h d", h=1), kr_tm2[f][:], 1,
                           RT["cos"][:, j * 16:(j + 1) * 16], RT["sin"][:, j * 16:(j + 1) * 16],
                           (ktmp[0][:], ktmp[1][:]), [psb[pp], Bt], Bk2[f], [Bk2[f]])
                if t >= 1:
                    d2(t - 1)
            d2(63)
            P.barrier()

        if phases >= 3:
          with ExitStack() as st:
            ckvT = sb(st, "ckvT", [128, 2, S], BF16)
            KT = sb(st, "KT", [128, S], BF16)
            VO = sb(st, "VO", [128, 64, 128], BF16)
            QT = [sb(st, f"QT{i}", [128, 2048], BF16) for i in range(2)]
            QTb = bufs(2)
            OBs = sb(st, "OBs", [64, 2048], BF16)
            wuk = sb(st, "wuk", [128, 2, 512], BF16)
            wuv = sb(st, "wuv", [128, 2, 512], BF16)
            kvn = sb(st, "kvn", [128, 2], F32)
            qidxB = sb(st, "qidxB", [128, 2048], F32)
            kidx = sb(st, "kidx", [128, 64], F32)
            EP = [sb(st, f"EP{i}", [128, 512], BF16) for i in range(6)]
            EPb = bufs(6)
            Bl, BKT, BVO, Bw3, BOBs = bufs(5)
            for c in range(2):
                O.dma('sp', ckvT[:, c, :], ckvT_d[c], [Bckv_d], [Bl])
            O.memset('pool', KT[96:128, :], 0.0, [BKT])
            O.dma('sp', KT[64:96, :], krT_d, [Bkr_d], [BKT])
            for qq in range(2):
                O.memset('pool', QT[qq][96:128, :], 0.0, [QTb[qq]])
            O.dma('sp', qidxB[:], A["qidx"], (), [Bc])
            O.dma('sp', kidx[:], A["kidx"], (), [Bc])
            O.dma('pool', wuk[:], A["w_uk"].rearrange("(c p) n -> p c n", p=128), (), [Bw3])
            O.dma('pool', wuv[:], A["w_uv"].rearrange("(c p) n -> p c n", p=128), (), [Bw3])
            O.dma('sp', kvn[:], A["kv_norm_col"], (), [Bw3])
            for c in range(2):
                O.act(wuk[:, c, :], wuk[:, c, :], AF.Copy, [Bw3], [Bw3], scale=kvn[:, c:c + 1])
                O.act(wuv[:, c, :], wuv[:, c, :], AF.Copy, [Bw3], [Bw3], scale=kvn[:, c:c + 1])
            O.memset('pool', VO[:, :, 64:128], 1.0, [BVO])
            tcount = 0
            for h in range(8):
                qs = h % 2
                O.dma('sp', QT[qs][0:96, :], qT_d[h], [BqT_d], [QTb[qs]])
                for n in range(16):
                    pb = 1 + n % 2
                    for c in range(2):
                        O.mm(ps[pb][0:64, :], wuk[:, c, h * 64:(h + 1) * 64], ckvT[:, c, n * 512:(n + 1) * 512],
                             c == 0, c == 1, [Bw3, Bl], [psb[pb]])
                    O.cp('act' if n % 2 == 0 else 'dve', KT[0:64, n * 512:(n + 1) * 512], ps[pb][0:64, :], [psb[pb]], [BKT])
                for n in range(8):
                    pb = (3, 0)[n % 2]
                    for kb8 in range(8):
                        kb = n * 8 + kb8
                        for c in range(2):
                            O.mm(ps[pb][:, kb8 * 64:(kb8 + 1) * 64], ckvT[:, c, kb * 128:(kb + 1) * 128],
                                 wuv[:, c, h * 64:(h + 1) * 64], c == 0, c == 1, [Bw3, Bl], [psb[pb]])
                    O.cp('dve' if n % 2 == 0 else 'act', VO[:, n * 8:(n + 1) * 8, 0:64],
                         ps[pb][:].rearrange("p (b d) -> p b d", b=8), [psb[pb]], [BVO])
                tiles = [(i, kb) for i in range(4) for kb in range(16 * (i + 1))]
                slots = {}
                LA = 3
                for n in range(len(tiles) + LA):
                    if n < len(tiles):
                        i, kb = tiles[n]
                        pbS = tcount % 6
                        slots[n] = pbS
                        tcount += 1
                        O.mm(ps[pbS][:], KT[:, kb * 128:(kb + 1) * 128], QT[qs][:, i * 512:(i + 1) * 512],
                             True, True, [BKT, QTb[qs]], [psb[pbS]])
                        O.act(EP[pbS][:], ps[pbS][:], AF.Exp, [psb[pbS]], [EPb[pbS]], scale=SCALE_MLA)
                        if kb >= 16 * i:
                            O.stt(EP[pbS][:], qidxB[:, i * 512:(i + 1) * 512], kidx[:, kb:kb + 1], EP[pbS][:],
                                  ALU.is_ge, ALU.mult, [Bc, EPb[pbS]], [EPb[pbS]])
                    m = n - LA
                    if m >= 0:
                        i, kb = tiles[m]
                        sl = slots[m]
                        nkb = 16 * (i + 1)
                        pacc = 6 + (i % 2)
                        O.mm(ps[pacc][:], VO[:, kb, :], EP[sl][:], kb == 0, kb == nkb - 1, [BVO, EPb[sl]], [psb[pacc]])
                        if kb == nkb - 1:
                            attn_epilogue(ps[pacc], psb[pacc], i % 2, OBs[:, i * 512:(i + 1) * 512], None, [BOBs])
                O.dma('sp', obT_d[h], OBs[:], [BOBs], [Bob_d])
            P.barrier()

        if phases >= 4:
          with ExitStack() as sX:
            x1 = sb(sX, "x1", [128, 16, D], F32)
            Bx1 = bufs(16)
            with ExitStack() as st:
                Wg = sb(st, "Wg", [128, 8, D], BF16)
                bWg = Buf()
                browg = sb(st, "browg", [1, D], BF16)
                load_win(Wg, bWg, browg, [(2464, 3488)])
                wbb = sb(st, "wbb", [64, 8, D], BF16)
                wout = sb(st, "wout", [128, 8, D], BF16)
                Bw4 = Buf()
                O.dma('pool', wbb[:], A["w_bb"].rearrange("(h p) n -> p h n", p=64), (), [Bw4])
                O.dma('pool', wout[:], A["w_out"].rearrange("(c p) n -> p c n", p=128), (), [Bw4])
                for k in range(8):
                    O.tt('dve', wout[:, k, :], wout[:, k, :], g1B[:], ALU.mult, [Bw4, Bmod], [Bw4])
                OBt = sb(st, "OBt", [64, 8, 128], BF16)
                zat = sb(st, "zat4", [128, D], BF16)
                sigb = sb(st, "sigb", [128, D], BF16)
                mixed = sb(st, "mixed", [128, D], BF16)
                mixT = sb(st, "mixT", [128, 8, 128], BF16)
                tmpf = sb(st, "tmpf", [128, D], F32)
                Bl4, Bm4 = bufs(2)
                pT6 = ps[6][:].bitcast(BF16)
                for ti in range(16):
                    i, r = ti // 4, ti % 4 + 1
                    front_end(A["xq"][i, r * 128:(r + 1) * 128, :], ti % 2)
                    O.dma('sp', OBt[:], obT_d[:, :, ti * 128:(ti + 1) * 128].rearrange("h p t -> p h t"), [Bob_d], [Bl4])
                    O.dma('sp', zat[:], za_d[ti], [Bza_d], [Bl4])
                    for hf in range(2):
                        pb = 1 + hf
                        c0 = hf * 512
                        for k in range(8):
                            O.mm(ps[pb][:], xhT[:, k, :], Wg[:, k, c0:c0 + 512], k == 0, False, [Bx, bWg], [psb[pb]])
                        O.mm(ps[pb][:], ones_b[0:1, :], browg[0:1, c0:c0 + 512], False, True, [Bc, bWg], [psb[pb]])
                        O.act(sigb[:, c0:c0 + 512], ps[pb][:], AF.Sigmoid, [psb[pb]], [Bm4])
                        pb2 = 3 + hf
                        for h in range(8):
                            O.mm(ps[pb2][:], OBt[:, h, :], wbb[:, h, c0:c0 + 512], h == 0, h == 7, [Bl4, Bw4], [psb[pb2]])
                        O.tt('dve', tmpf[:, c0:c0 + 512], ps[pb2][:], sigb[:, c0:c0 + 512], ALU.mult, [psb[pb2], Bm4], [Bm4])
                    O.tt('dve', mixed[:], tmpf[:], zat[:], ALU.add, [Bm4, Bl4], [Bm4])
                    for k in range(8):
                        O.tr(pT6[:, k * 128:(k + 1) * 128], mixed[:, k * 128:(k + 1) * 128], ident_b[:], [Bm4, Bc], [psb[6]])
                    O.cp('act', mixT[:].rearrange("p k t -> p (k t)"), pT6, [psb[6]], [Bm4])
                    for hf in range(2):
                        pb = 1 + hf
                        c0 = hf * 512
                        for k in range(8):
                            O.mm(ps[pb][:], mixT[:, k, :], wout[:, k, c0:c0 + 512], k == 0, k == 7, [Bm4, Bw4], [psb[pb]])
                        O.tt('dve', x1[:, ti, c0:c0 + 512], ps[pb][:], xt[ti % 2][:, c0:c0 + 512], ALU.add,
                             [psb[pb], xtb[ti % 2]], [Bx1[ti]])
                P.barrier()

            h2 = sb(sX, "h2", [128, 16, D], BF16)
            Bh2 = bufs(16)
            gwb = sb(sX, "gwb", [128, 16, 32], BF16)
            posm = sb(sX, "posm", [128, 16, 32], F32)
            maskb = sb(sX, "maskb", [128, 16, 32], BF16)
            Brt = Buf()
            with ExitStack() as st:
                wr = sb(st, "wr", [128, 8, 32], F32)
                brB = sb(st, "brB", [128, 32], F32)
                ut = sb(st, "ut", [128, 128], F32)
                utb = sb(st, "utb", [128, 128], BF16)
                Bw4 = Buf()
                O.dma('sp', wr[:], A["w_router"].rearrange("(c p) n -> p c n", p=128), (), [Bw4])
                O.dma('sp', brB[:], A["b_router_b"], (), [Bw4])
                O.dma('sp', ut[:], A["ut"], (), [Bw4])
                O.cp('dve', utb[:], ut[:], [Bw4], [Bw4])
                tmpf = sb(st, "tmpf2", [128, D], F32)
                b2all = sb(st, "b2all", [32, D], BF16)
                gwT = sb(st, "gwT", [32, 128], BF16)
                Bb2 = Buf()
                O.dma('pool', b2all[:], A["b_moe2"], (), [Bw4])
                h2f = sb(st, "h2f", [128, D], F32)
                h2fT = sb(st, "h2fT", [128, 8, 128], F32)
                lg = sb(st, "lg", [128, 32], F32)
                ex = sb(st, "ex", [128, 32], F32)
                mk = sb(st, "mk", [128, 32], F32)
                m8 = sb(st, "m8", [128, 8], F32)
                s4 = sb(st, "s4", [128, 8], F32)
                Bm4, Br4 = bufs(2)
                for ti in range(16):
                    rstd_of(x1[:, ti, :], D, junk[:], sml[:, 2:3], sml[:, 4:5], [Bx1[ti]], Br4)
                    O.stt(tmpf[:], x1[:, ti, :], sml[:, 4:5], G2B[:], ALU.mult, ALU.mult, [Bx1[ti], Br4, Bmod, Bm4], [Bm4])
                    O.tt('dve', h2f[:], tmpf[:], sh2B[:], ALU.add, [Bm4, Bmod], [Br4])
                    O.cp('act', h2[:, ti, :], h2f[:], [Br4], [Bh2[ti]])
                    for half in range(2):
                        pb = 3 + half
                        for k4 in range(4):
                            k = half * 4 + k4
                            O.tr(ps[pb][:, k4 * 128:(k4 + 1) * 128], h2f[:, k * 128:(k + 1) * 128], ident_f[:], [Br4, Bc], [psb[pb]])
                        O.cp('act' if half == 0 else 'dve', h2fT[:, half * 4:(half + 1) * 4, :].rearrange("p k t -> p (k t)"),
                             ps[pb][:], [psb[pb]], [Br4])
                    for k in range(8):
                        O.mm(ps[5][:, 0:32], h2fT[:, k, :], wr[:, k, :], k == 0, k == 7, [Br4, Bw4], [psb[5]])
                    O.tt('dve', lg[:], ps[5][:, 0:32], brB[:], ALU.add, [psb[5], Bw4], [Br4])
                    O.max8(m8[:], lg[:], [Br4], [Br4])
                    O.ts('dve', mk[:], lg[:], m8[:, 3:4], None, ALU.is_ge, None, [Br4], [Br4])
                    O.ts('dve', s4[:, 0:1], m8[:, 0:1], -1.0, None, ALU.mult, None, [Br4], [Br4])
                    O.act(ex[:], lg[:], AF.Exp, [Br4], [Br4], bias=s4[:, 0:1])
                    O.stt(ex[:], ex[:], 1.0, mk[:], ALU.mult, ALU.mult, [Br4], [Br4], accum_out=s4[:, 1:2])
                    O.recip(s4[:, 2:3], s4[:, 1:2], [Br4], [Br4])
                    O.ts('dve', gwb[:, ti, :], ex[:], s4[:, 2:3], None, ALU.mult, None, [Br4], [Brt])
                    O.cp('dve', maskb[:, ti, :], mk[:], [Br4], [Brt])
                    g0 = (ti // 4) * 4
                    for a in range(g0, ti + 1):
                        O.mm(ps[5][:, 32:64], (ones_b[:] if a < ti else utb[:]), maskb[:, a, :], a == g0, a == ti,
                             [Bc, Bw4, Brt], [psb[5]])
                    O.stt(ex[:], ps[5][:, 32:64], 1.0, mk[:], ALU.add, ALU.mult, [psb[5], Br4], [Br4])
                    O.ts('dve', posm[:, ti, :], ex[:], -1.0, None, ALU.add, None, [Br4], [Brt])
                    pT7b = ps[7][:].bitcast(BF16)
                    O.tr(pT7b[0:32, 0:128], gwb[:, ti, :], ident_b[:], [Brt, Bc], [psb[7]])
                    O.cp('act', gwT[:], pT7b[0:32, 0:128], [psb[7]], [Bb2])
                    for hf in range(2):
                        pb = 1 + hf
                        O.mm(ps[pb][:], gwT[:], b2all[:, hf * 512:(hf + 1) * 512], True, True, [Bb2, Bw4], [psb[pb]])
                        O.tt('dve', tmpf[:, hf * 512:(hf + 1) * 512], ps[pb][:], g2B[:, hf * 512:(hf + 1) * 512], ALU.mult,
                             [psb[pb], Bmod, Bm4, Br4], [Bm4])
                        O.tt('dve', x1[:, ti, hf * 512:(hf + 1) * 512], x1[:, ti, hf * 512:(hf + 1) * 512],
                             tmpf[:, hf * 512:(hf + 1) * 512], ALU.add, [Bm4, Bx1[ti], Bh2[ti]], [Bx1[ti]])
                P.barrier()

            with ExitStack() as st:
                ring = [sb(st, f"ring{i}", [128, 8, 512], BF16) for i in range(6)]
                ringb = bufs(6)
                iota = sh2B[:, 0:256]
                Sel = g1B[:].bitcast(BF16).rearrange("p (a j) -> p a j", a=8)
                SelT = G2B[:].bitcast(BF16).rearrange("p (g t) -> p g t", g=4)
                xg = sb(st, "xg", [128, 8, 512], BF16)
                actT = sb(st, "actT", [128, 8, 512], BF16)
                ysb3 = xh_1
                ysb = [xh[:], junk[:], xhT[:].rearrange("p k t -> p (k t)"), ysb3[:]]
                _al = xhT_1[:].rearrange("p k t -> p (k t)").bitcast(F32)
                wj = _al[:, 0:4]
                b1c = [_al[:, 16:32], _al[:, 32:48]]
                b2r = [E["den"][0][0:1, :].bitcast(BF16), E["den"][1][0:1, :].bitcast(BF16)]
                bb = bufs(2)
                SW = [(E["ot"][0][:], E["ot"][1][:], xt[0][:, 0:512]), (xt[0][:, 512:1024], xt[1][:, 0:512], xt[1][:, 512:1024])]
                Bsw2 = bufs(2)
                BSel, BSelT, Bxg, Bact, By, Bwj, Bsw = bufs(7)
                O.dma('sp', iota, A["iota_j"], (), [Bc])
                w1v = A["w_moe1"].rearrange("e (c p) n -> e p c n", p=128)
                w2v = A["w_moe2"].rearrange("e (c p) n -> e p c n", p=128)
                rr = 0
                pT6 = ps[6][:].bitcast(BF16)

                def load_piece(sl, src):
                    O.dma('pool', ring[sl][:, 0:4, :], src[:, 0:4, :], (), [ringb[sl]])
                    O.dma('pool', ring[sl][:, 4:8, :], src[:, 4:8, :], (), [ringb[sl]])
                wj2 = [_al[:, 0:4], _al[:, 8:12]]
                Bwj2 = bufs(2)
                G = 2 * n_experts
                wslots = {}

                def selb(g):
                    e, half = g // 2, g % 2
                    for a8 in range(8):
                        a = half * 8 + a8
                        O.ts('dve', Sel[:, a8, :], iota, posm[:, a, e:e + 1], None, ALU.is_equal, None, [Bc, Brt], [BSel])

                def wjg(g):
                    e, half = g // 2, g % 2
                    for gi2 in range(2):
                        for jc in range(2):
                            ch = gi2 * 2 + jc
                            for a4 in range(4):
                                a8 = gi2 * 4 + a4
                                a = half * 8 + a8
                                O.mm(ps[7][:, ch:ch + 1], Sel[:, a8, jc * 128:(jc + 1) * 128], gwb[:, a, e:e + 1],
                                     a4 == 0, a4 == 3, [BSel, Brt], [psb[7]])
                    O.cp('act', wj2[g % 2], ps[7][:, 0:4], [psb[7]], [Bwj2[g % 2]])
                    for c in range(8):
                        pb = (0, 1, 6, 7)[c % 4]
                        for gi2 in range(2):
                            for a4 in range(4):
                                a8 = gi2 * 4 + a4
                                a = half * 8 + a8
                                O.mm(ps[pb][:, gi2 * 256:(gi2 + 1) * 256], h2[:, a, c * 128:(c + 1) * 128], Sel[:, a8, :],
                                     a4 == 0, a4 == 3, [Bh2[a], BSel], [psb[pb]])
                        O.cp('act', xg[:, c, :], ps[pb][:], [psb[pb]], [Bxg])

                def selT(g):
                    for gi2 in range(2):
                        for jc in range(2):
                            ch = gi2 * 2 + jc
                            for a4 in range(4):
                                O.tr(pT6[:, a4 * 128:(a4 + 1) * 128], Sel[:, gi2 * 4 + a4, jc * 128:(jc + 1) * 128], ident_b[:],
                                     [BSel, Bc], [psb[6]])
                            O.cp('act', SelT[:, ch, :], pT6[:, 0:512], [psb[6]], [BSelT])

                def loads_A(e):
                    es = e % 2
                    load_piece(0, w1v[e][:, :, 0:512])
                    load_piece(1, w1v[e][:, :, 1024:1536])
                    O.dma('sp', b1c[es], A["b1c"][e], (), [bb[es]])

                def loads_B(e):
                    load_piece(2, w1v[e][:, :, 512:1024])
                    load_piece(3, w1v[e][:, :, 1536:2048])

                def loads_2(e, hf):
                    load_piece(4 + hf, w2v[e][:, :, hf * 512:(hf + 1) * 512])

                loads_A(0)
                loads_B(0)
                loads_2(0, 0)
                loads_2(0, 1)
                selb(0)
                wjg(0)
                selT(0)
                for g in range(G):
                    e, half = g // 2, g % 2
                    es = e % 2
                    wj = wj2[g % 2]
                    Bwj = Bwj2[g % 2]
                    if g + 1 < G:
                        selb(g + 1)
                    for m in range(8):
                        pg, pl = (2, 3) if m % 2 == 0 else (4, 5)
                        gt, sg, lr = SW[m % 2]
                        s_g, s_l = (0, 1) if m < 4 else (2, 3)
                        mc = (m % 4) * 128
                        for c in range(8):
                            O.mm(ps[pg][:], ring[s_g][:, c, mc:mc + 128], xg[:, c, :], c == 0, c == 7,
                                 [ringb[s_g], Bxg], [psb[pg]])
                        for c in range(8):
                            O.mm(ps[pl][:], ring[s_l][:, c, mc:mc + 128], xg[:, c, :], c == 0, c == 7,
                                 [ringb[s_l], Bxg], [psb[pl]])
                        Bs_ = Bsw2[m % 2]
                        O.ts('dve', gt, ps[pg][:], b1c[es][:, m:m + 1], 7.0, ALU.add, ALU.min, [psb[pg], bb[es]], [Bs_])
                        O.act(lr, ps[pl][:], AF.Identity, [psb[pl], bb[es]], [Bs_], bias=b1c[es][:, 8 + m:9 + m])
                        O.act(sg, gt, AF.Sigmoid, [Bs_], [Bs_], scale=1.702)
                        O.ts('dve', lr, lr, 7.0, -7.0, ALU.min, ALU.max, [Bs_], [Bs_])
                        O.tt('dve', gt, gt, sg, ALU.mult, [Bs_], [Bs_])
                        O.stt(actT[:, m, :], lr, 1.0, gt, ALU.add, ALU.mult, [Bs_], [Bact])
                        if m == 3 and half == 1 and e + 1 < n_experts:
                            loads_A(e + 1)
                    if half == 1 and e + 1 < n_experts:
                        loads_B(e + 1)
                    if g + 1 < G:
                        wjg(g + 1)
                    for hf in range(2):
                        for ch in range(4):
                            pb = 6 + ch % 2
                            for m in range(8):
                                O.mm(ps[pb][:], actT[:, m, ch * 128:(ch + 1) * 128], ring[4 + hf][:, m, :],
                                     m == 0, m == 7, [Bact, ringb[4 + hf]], [psb[pb]])
                            O.stt(ysb[ch][:, hf * 512:(hf + 1) * 512], ps[pb][:], wj[:, ch:ch + 1], g2B[:, hf * 512:(hf + 1) * 512],
                                  ALU.mult, ALU.mult, [psb[pb], Bwj, Bmod], [By])
                        if half == 1 and e + 1 < n_experts:
                            loads_2(e + 1, hf)
                    for a8 in range(8):
                        a = half * 8 + a8
                        gi2, a4 = a8 // 4, a8 % 4
                        for hf in range(2):
                            pb = (a8 * 2 + hf) % 6
                            for jc in range(2):
                                ch = gi2 * 2 + jc
                                O.mm(ps[pb][:], SelT[:, ch, a4 * 128:(a4 + 1) * 128], ysb[ch][:, hf * 512:(hf + 1) * 512],
                                     jc == 0, jc == 1, [BSelT, By], [psb[pb]])
                            O.tt('dve', x1[:, a, hf * 512:(hf + 1) * 512], x1[:, a, hf * 512:(hf + 1) * 512], ps[pb][:],
                                 ALU.add, [psb[pb], Bx1[a]], [Bx1[a]])
                    if g + 1 < G:
                        selT(g + 1)
                P.barrier()

            with ExitStack() as st:
                fnB = sb(st, "fnB", [128, D], F32)
                ob = [sb(st, f"ob{i}", [128, D], F32) for i in range(2)]
                obb = bufs(2)
                Bf = Buf()
                O.dma('sp', fnB[:], A["final_norm_b"], (), [Bf])
                outs = []
                for ti in range(16):
                    sl = ti % 2
                    rstd_of(x1[:, ti, :], D, junk[:], sml[:, 2:3], sml[:, 4:5], [Bx1[ti]], Bf)
                    O.stt(ob[sl][:], x1[:, ti, :], sml[:, 4:5], fnB[:], ALU.mult, ALU.mult, [Bx1[ti], Bf], [obb[sl]])
                    outs.append(O.dma('sp', out_d[ti * 128:(ti + 1) * 128, :], ob[sl][:], [obb[sl]], ()))
                P.wait_all('sp', outs)
        P.barrier()
        P.finish()
    return nc


def _host_inputs(inputs):
    f32 = np.float32
    x = np.asarray(inputs["x"], f32)
    c = np.asarray(inputs["c"], f32)
    pos = np.asarray(inputs["positions"], np.int32)
    rep = lambda v: np.ascontiguousarray(np.broadcast_to(np.asarray(v, f32).reshape(1, -1), (128, np.asarray(v).size)))
    col = lambda v: np.ascontiguousarray(np.asarray(v, f32).reshape(-1, 128).T)
    shared = {
        "w_ada": np.ascontiguousarray(inputs["w_ada"][0], f32), "b_ada_b": rep(inputs["b_ada"][0]),
        "norm_mix_col": col(inputs["norm_mix"][0]), "norm_ffn_b": rep(inputs["norm_ffn"][0]),
        "final_norm_b": rep(inputs["final_norm"]), "w_in": np.ascontiguousarray(inputs["w_in"][0], f32),
        "sinks_b": rep(inputs["sinks"][0]), "q_norm_col": col(inputs["q_norm"][0]), "kv_norm_col": col(inputs["kv_norm"][0]),
        "w_uq": np.ascontiguousarray(inputs["w_uq"][0], f32), "w_uk": np.ascontiguousarray(inputs["w_uk"][0], f32),
        "w_uv": np.ascontiguousarray(inputs["w_uv"][0], f32), "w_ba": np.ascontiguousarray(inputs["w_branch_a"][0], f32),
        "w_bb": np.ascontiguousarray(inputs["w_branch_b"][0], f32), "w_out": np.ascontiguousarray(inputs["w_out"][0], f32),
        "w_router": np.ascontiguousarray(inputs["w_router"][0], f32), "b_router_b": rep(inputs["b_router"][0]),
        "w_moe1": np.ascontiguousarray(inputs["w_moe1"][0], f32),
        "b1c": np.ascontiguousarray(np.asarray(inputs["b_moe1"][0], f32).reshape(32, 16, 128).transpose(0, 2, 1)),
        "w_moe2": np.ascontiguousarray(inputs["w_moe2"][0], f32), "b_moe2": np.ascontiguousarray(inputs["b_moe2"][0], f32),
    }
    shared["ident"] = np.eye(128, dtype=f32)
    t = np.arange(128)
    shared["ut"] = (t[:, None] < t[None, :]).astype(f32)
    shared["iota_j"] = np.ascontiguousarray(np.broadcast_to(np.arange(256, dtype=f32)[None, :], (128, 256)))
    slopes = np.exp2(-8.0 * np.arange(1, 9) / 8).astype(f32)
    k = t[:, None].astype(f32)
    q = t[None, :].astype(f32)
    tab = np.zeros((128, 2, 8, 128), f32)
    for typ in range(2):
        dist = q - k + (128.0 if typ == 0 else 0.0)
        valid = (dist >= 0) & (dist < 128)
        for h in range(8):
            tab[:, typ, h, :] = np.where(valid, -slopes[h] * dist, NEG)
    shared["swa_tab"] = tab.reshape(128, -1)
    freqs = (10000.0 ** (-np.arange(0, 32, 2, dtype=f32) / f32(32))).astype(f32)
    shared["freq_b"] = np.ascontiguousarray(np.broadcast_to(np.tile(freqs, 4)[None, :], (128, 64)))
    shared["kidx"] = np.ascontiguousarray((np.arange(64)[None, :] * 128 + t[:, None]).astype(f32))
    in_maps = []
    own = []
    for core in range(NCORES):
        b, j = core // 4, core % 4
        sbs = [j, 7 - j, 8 + j, 15 - j]
        xq = np.zeros((4, 640, D), f32)
        tok = []
        halo = np.zeros((128, 16), f32)
        for i, s in enumerate(sbs):
            st_ = s * 512
            if st_ >= 128:
                xq[i, 0:128] = x[b, st_ - 128:st_]
            else:
                halo[:, 4 * i] = NEG
            xq[i, 128:640] = x[b, st_:st_ + 512]
            tok.append(np.arange(st_, st_ + 512))
        tok = np.concatenate(tok)
        own.append((b, tok))
        m = dict(shared)
        m["xq"] = xq
        m["xall"] = np.ascontiguousarray(x[b])
        m["posq"] = np.ascontiguousarray(pos[b, tok].reshape(16, 128).T)
        m["posall"] = np.ascontiguousarray(pos[b].reshape(64, 128).T)
        m["qidx"] = np.ascontiguousarray(np.broadcast_to(tok.astype(f32)[None, :], (128, 2048)))
        m["halo_bias"] = halo
        m["c_col"] = col(c[b])
        in_maps.append(m)
    return in_maps, own


_NC_CACHE = {}


def kernel(**inputs):
    in_maps, own = _host_inputs(inputs)
    if "nc" not in _NC_CACHE:
        _NC_CACHE["nc"] = build_program()
    res = run_bass_kernel_spmd(_NC_CACHE["nc"], in_maps, core_ids=list(range(NCORES)))
    out = np.zeros((2, S, D), np.float32)
    for core in range(NCORES):
        b, tok = own[core]
        out[b, tok] = res.results[core]["out"]
    return out
```
